# Optimizing a Trainium2 kernel written in Bass

```python
import jax, jax.numpy as jnp
from jax import lax
import numpy as np

D_MODEL = 1024
BATCH = 8
SEQ = 2048
DEPTH = 2

N_MIXERS = 2
N_A_LAYERS = (DEPTH + 1) // 2
N_B_LAYERS = DEPTH // 2
EPS = 1e-6
NEG_INF = -1e30

GLA_HEADS = 4
GLA_DK = D_MODEL // 2
GLA_DV = D_MODEL
GLA_HK = GLA_DK // GLA_HEADS
GLA_HV = GLA_DV // GLA_HEADS
GLA_GATE_RANK = 16
GLA_GATE_NORMALIZER = 16.0
GLA_CHUNK = 64
GLA_SPLITS = (GLA_DK, 2 * GLA_DK, 2 * GLA_DK + GLA_DV, 2 * GLA_DK + 2 * GLA_DV)
GLA_IN = 2 * GLA_DK + 2 * GLA_DV + GLA_GATE_RANK

MOBA_HEADS = 8
MOBA_HD = D_MODEL // MOBA_HEADS
MOBA_BLOCK = 256
MOBA_TOPK = 3
MOBA_QCHUNK = 32

D_FF = D_MODEL * 7 // 2
N_EXPERTS = 8
TOP_K = 2

kernel_name = "hybrid_gla_moba_moe_trunk"


def rms_norm(x, gain):
    xf = x.astype(jnp.float32)
    y = xf * lax.rsqrt(jnp.mean(xf * xf, axis=-1, keepdims=True) + EPS)
    return (y * gain.astype(jnp.float32)).astype(x.dtype)


def gla_mixer(h, w_in, w_gate2, b_gate, out_gain, w_out):
    B, S, _ = h.shape
    n_chunks = S // GLA_CHUNK
    q, k, v, g, a_lr = jnp.split(h @ w_in, GLA_SPLITS, axis=-1)
    log_a = jax.nn.log_sigmoid((a_lr @ w_gate2 + b_gate).astype(jnp.float32)) / GLA_GATE_NORMALIZER

    def to_chunks(t, hd):
        return t.astype(jnp.float32).reshape(B, n_chunks, GLA_CHUNK, GLA_HEADS, hd).transpose(0, 3, 1, 2, 4)

    q = to_chunks(q, GLA_HK) * (GLA_HK ** -0.5)
    k = to_chunks(k, GLA_HK)
    v = to_chunks(v, GLA_HV)
    cum = jnp.cumsum(to_chunks(log_a, GLA_HK), axis=3)
    q_dec = q * jnp.exp(cum)
    k_inv = k * jnp.exp(-cum)
    causal = jnp.tril(jnp.ones((GLA_CHUNK, GLA_CHUNK), dtype=bool))
    scores = jnp.where(causal, jnp.einsum('bhnid,bhnjd->bhnij', q_dec, k_inv), 0.0)
    o_intra = jnp.einsum('bhnij,bhnjv->bhniv', scores, v)
    cum_last = cum[:, :, :, -1, :]
    k_to_end = k * jnp.exp(cum_last[:, :, :, None, :] - cum)
    chunk_kv = jnp.einsum('bhncd,bhncv->bhndv', k_to_end, v)

    def step(state, inp):
        decay, kv = inp
        return decay[..., None] * state + kv, state

    init = jnp.zeros((B, GLA_HEADS, GLA_HK, GLA_HV), jnp.float32)
    _, prev_states = lax.scan(step, init, (jnp.exp(cum_last).transpose(2, 0, 1, 3),
                                           chunk_kv.transpose(2, 0, 1, 3, 4)))
    o_inter = jnp.einsum('bhncd,nbhdv->bhncv', q_dec, prev_states)
    o = rms_norm(o_intra + o_inter, out_gain)
    o = o.transpose(0, 2, 3, 1, 4).reshape(B, S, GLA_DV)
    o = o * jax.nn.silu(g.astype(jnp.float32))
    return o.astype(h.dtype) @ w_out


def moba_mixer(h, w_qkv, q_gain, k_gain, w_out):
    B, S, _ = h.shape
    n_blocks = -(-S // MOBA_BLOCK)
    s_pad = n_blocks * MOBA_BLOCK
    n_qchunks = s_pad // MOBA_QCHUNK
    top_k = min(MOBA_TOPK, n_blocks)
    q, k, v = jnp.split(h @ w_qkv, 3, axis=-1)

    def to_heads(t, gain):
        t = t.astype(jnp.float32).reshape(B, S, MOBA_HEADS, MOBA_HD)
        if gain is not None:
            t = rms_norm(t, gain)
        t = t.transpose(0, 2, 1, 3)
        return jnp.pad(t, ((0, 0), (0, 0), (0, s_pad - S), (0, 0)))

    q = to_heads(q, q_gain)
    k = to_heads(k, k_gain)
    v = to_heads(v, None)
    kb = k.reshape(B, MOBA_HEADS, n_blocks, MOBA_BLOCK, MOBA_HD)
    vb = v.reshape(B, MOBA_HEADS, n_blocks, MOBA_BLOCK, MOBA_HD)
    k_mean = kb.mean(axis=3)

    q_block = jnp.arange(s_pad) // MOBA_BLOCK
    fully_past = jnp.arange(n_blocks)[None, :] < q_block[:, None]
    gate = jnp.where(fully_past, jnp.einsum('bhsd,bhnd->bhsn', q, k_mean), NEG_INF)
    _, sel = lax.top_k(gate, top_k)
    slot_valid = jnp.arange(top_k)[None, :] < q_block[:, None]

    q_c = q.reshape(B, MOBA_HEADS, n_qchunks, MOBA_QCHUNK, MOBA_HD).transpose(2, 0, 1, 3, 4)
    sel_c = sel.reshape(B, MOBA_HEADS, n_qchunks, MOBA_QCHUNK, top_k).transpose(2, 0, 1, 3, 4)
    valid_c = slot_valid.reshape(n_qchunks, MOBA_QCHUNK, top_k)
    b_idx = jnp.arange(B)[:, None, None, None]
    h_idx = jnp.arange(MOBA_HEADS)[None, :, None, None]
    scale = MOBA_HD ** -0.5
    n_sel = top_k * MOBA_BLOCK

    def attend(args):
        qc, selc, validc, ci = args
        k_sel = kb[b_idx, h_idx, selc].reshape(B, MOBA_HEADS, MOBA_QCHUNK, n_sel, MOBA_HD)
        v_sel = vb[b_idx, h_idx, selc].reshape(B, MOBA_HEADS, MOBA_QCHUNK, n_sel, MOBA_HD)
        s_sel = jnp.einsum('bhqd,bhqjd->bhqj', qc, k_sel) * scale
        s_sel = jnp.where(jnp.repeat(validc, MOBA_BLOCK, axis=-1), s_sel, NEG_INF)
        own = (ci * MOBA_QCHUNK) // MOBA_BLOCK
        k_own = lax.dynamic_index_in_dim(kb, own, axis=2, keepdims=False)
        v_own = lax.dynamic_index_in_dim(vb, own, axis=2, keepdims=False)
        s_own = jnp.einsum('bhqd,bhjd->bhqj', qc, k_own) * scale
        q_pos = ci * MOBA_QCHUNK + jnp.arange(MOBA_QCHUNK)
        k_pos = own * MOBA_BLOCK + jnp.arange(MOBA_BLOCK)
        s_own = jnp.where(k_pos[None, :] <= q_pos[:, None], s_own, NEG_INF)
        p = jax.nn.softmax(jnp.concatenate([s_sel, s_own], axis=-1), axis=-1)
        return (jnp.einsum('bhqj,bhqjd->bhqd', p[..., :n_sel], v_sel)
                + jnp.einsum('bhqj,bhjd->bhqd', p[..., n_sel:], v_own))

    o = lax.map(attend, (q_c, sel_c, valid_c, jnp.arange(n_qchunks)))
    o = o.transpose(1, 0, 3, 2, 4).reshape(B, s_pad, MOBA_HEADS * MOBA_HD)[:, :S]
    return o.astype(h.dtype) @ w_out


def swiglu(h, w_gate, w_up, w_down):
    return (jax.nn.silu(h @ w_gate) * (h @ w_up)) @ w_down


def moe_swiglu(h, w_router, w_gate, w_up, w_down):
    logits = (h @ w_router).astype(jnp.float32)
    top_val, top_idx = lax.top_k(logits, TOP_K)
    weights = jax.nn.softmax(top_val, axis=-1)
    gates = jnp.sum(jax.nn.one_hot(top_idx, N_EXPERTS, dtype=jnp.float32) * weights[..., None], axis=-2)
    gates = gates.astype(h.dtype)
    out = jnp.zeros_like(h)
    for e in range(N_EXPERTS):
        out = out + gates[..., e:e + 1] * swiglu(h, w_gate[e], w_up[e], w_down[e])
    return out


def setup_inputs(seed: int = 0) -> dict:
    key = jax.random.key(seed)
    ks = iter(jax.random.split(key, 24))

    def nrm(shape, fan_in, scale=1.0):
        return jax.random.normal(next(ks), shape, jnp.float32) * (scale * fan_in ** -0.5)

    def gain(shape):
        return 1.0 + 0.02 * jax.random.normal(next(ks), shape, jnp.float32)

    res_scale = (2 * DEPTH) ** -0.5
    return {
        "x": jax.random.normal(next(ks), (BATCH, SEQ, D_MODEL), jnp.float32),
        "norm_mix": gain((DEPTH, D_MODEL)),
        "norm_ffn": gain((DEPTH, D_MODEL)),
        "gla_w_in": nrm((N_A_LAYERS, D_MODEL, GLA_IN), D_MODEL),
        "gla_w_gate2": nrm((N_A_LAYERS, GLA_GATE_RANK, GLA_DK), GLA_GATE_RANK),
        "gla_b_gate": 0.1 * jax.random.normal(next(ks), (N_A_LAYERS, GLA_DK), jnp.float32),
        "gla_out_gain": gain((N_A_LAYERS, GLA_HV)),
        "gla_w_out": nrm((N_A_LAYERS, GLA_DV, D_MODEL), GLA_DV, res_scale),
        "moba_w_qkv": nrm((N_B_LAYERS, D_MODEL, 3 * MOBA_HEADS * MOBA_HD), D_MODEL),
        "moba_q_gain": gain((N_B_LAYERS, MOBA_HD)),
        "moba_k_gain": gain((N_B_LAYERS, MOBA_HD)),
        "moba_w_out": nrm((N_B_LAYERS, MOBA_HEADS * MOBA_HD, D_MODEL), MOBA_HEADS * MOBA_HD, res_scale),
        "ffn_w_gate": nrm((N_A_LAYERS, D_MODEL, D_FF), D_MODEL),
        "ffn_w_up": nrm((N_A_LAYERS, D_MODEL, D_FF), D_MODEL),
        "ffn_w_down": nrm((N_A_LAYERS, D_FF, D_MODEL), D_FF, res_scale),
        "moe_w_router": nrm((N_B_LAYERS, D_MODEL, N_EXPERTS), D_MODEL),
        "moe_w_gate": nrm((N_B_LAYERS, N_EXPERTS, D_MODEL, D_FF), D_MODEL),
        "moe_w_up": nrm((N_B_LAYERS, N_EXPERTS, D_MODEL, D_FF), D_MODEL),
        "moe_w_down": nrm((N_B_LAYERS, N_EXPERTS, D_FF, D_MODEL), D_FF, res_scale),
    }


def reference(x, norm_mix, norm_ffn, gla_w_in, gla_w_gate2, gla_b_gate, gla_out_gain, gla_w_out,
              moba_w_qkv, moba_q_gain, moba_k_gain, moba_w_out, ffn_w_gate, ffn_w_up, ffn_w_down,
              moe_w_router, moe_w_gate, moe_w_up, moe_w_down):
    for i in range(DEPTH):
        j = i // N_MIXERS
        h = rms_norm(x, norm_mix[i])
        if i % N_MIXERS == 0:
            x = x + gla_mixer(h, gla_w_in[j], gla_w_gate2[j], gla_b_gate[j], gla_out_gain[j], gla_w_out[j])
        else:
            x = x + moba_mixer(h, moba_w_qkv[j], moba_q_gain[j], moba_k_gain[j], moba_w_out[j])
        h = rms_norm(x, norm_ffn[i])
        if i % 2 == 0:
            x = x + swiglu(h, ffn_w_gate[j], ffn_w_up[j], ffn_w_down[j])
        else:
            x = x + moe_swiglu(h, moe_w_router[j], moe_w_gate[j], moe_w_up[j], moe_w_down[j])
    return x
```

```python
import numpy as np
from contextlib import ExitStack
import concourse.bass as bass
import concourse.mybir as mybir
from concourse.bass_utils import run_bass_kernel_spmd

F32 = mybir.dt.float32
BF16 = mybir.dt.bfloat16
AF = mybir.ActivationFunctionType
ALU = mybir.AluOpType
AX = mybir.AxisListType

S = 2048
D = 1024
NCH = 8
DFF = 3584
NE = 8
EPS = 1e-6
NEG = -30000.0
SEM_CH = 20000


class Buf:
    __slots__ = ("name", "last_w", "readers")

    def __init__(self, name):
        self.name = name
        self.last_w = None
        self.readers = []


class Op:
    __slots__ = ("eng", "fn", "deps", "signal", "sig", "dma_key", "dval")

    def __init__(self, eng, fn, dma_key):
        self.eng = eng
        self.fn = fn
        self.deps = []
        self.signal = False
        self.sig = 0
        self.dma_key = dma_key
        self.dval = 0


class Sched:
    ENGS = ("pe", "act", "dve", "pool", "sp")

    def __init__(self):
        self.ops = {e: [] for e in self.ENGS}
        self.dma_cnt = {}
        self.last_dma = {}

    def op(self, eng, fn, reads=(), writes=(), dma_key=None):
        o = Op(eng, fn, dma_key)
        deps = {}

        def add(p, kind):
            if p is None:
                return
            if p.dma_key is None and o.dma_key is None and p.eng == eng:
                if eng == "pe":
                    return
            deps[id(p)] = p

        for b in reads:
            add(b.last_w, "raw")
        for b in writes:
            add(b.last_w, "waw")
            for r in b.readers:
                add(r, "war")
        o.deps = list(deps.values())
        for p in o.deps:
            p.signal = True
        for b in reads:
            if o.dma_key is None:
                b.readers = [r for r in b.readers if not (r.dma_key is None and r.eng == eng)]
            b.readers.append(o)
        for b in writes:
            b.last_w = o
            b.readers = []
        if dma_key is not None:
            self.dma_cnt[dma_key] = self.dma_cnt.get(dma_key, 0) + 16
            o.dval = self.dma_cnt[dma_key]
            self.last_dma[dma_key] = o
        self.ops[eng].append(o)
        return o

    def barrier(self):
        lasts = []
        for e in self.ENGS:
            for p in reversed(self.ops[e]):
                if p.dma_key is None and p.fn is not None:
                    lasts.append(p)
                    break
        lasts += list(self.last_dma.values())
        for e in self.ENGS:
            o = Op(e, None, None)
            o.deps = [p for p in lasts if not (p.dma_key is None and p.eng == e)]
            for p in o.deps:
                p.signal = True
            self.ops[e].append(o)

    def finalize(self):
        self.nsig = {}
        for e in self.ENGS:
            n = 0
            for o in self.ops[e]:
                if o.dma_key is None and o.signal:
                    n += 1
                    o.sig = n
            self.nsig[e] = n

    def emit(self, nc, es, block):
        self.finalize()
        engsem = {}
        for e in self.ENGS:
            nchunks = (self.nsig[e] + SEM_CH - 1) // SEM_CH
            engsem[e] = [es.enter_context(nc.semaphore(f"s_{e}_{i}")) for i in range(max(nchunks, 1))]
        dmasem = {k: es.enter_context(nc.semaphore(f"d_{k}")) for k in self.dma_cnt}
        handles = {"pe": nc.tensor, "act": nc.scalar, "dve": nc.vector, "pool": nc.gpsimd, "sp": nc.sync}

        def semval(p):
            if p.dma_key is not None:
                return dmasem[p.dma_key], p.dval
            return engsem[p.eng][(p.sig - 1) // SEM_CH], (p.sig - 1) % SEM_CH + 1

        def run(e):
            h = handles[e]
            waited = {}
            for o in self.ops[e]:
                need = {}
                for p in o.deps:
                    s, v = semval(p)
                    k = id(s)
                    if v > waited.get(k, 0) and v > need.get(k, (None, 0))[1]:
                        need[k] = (s, v)
                if o.dma_key is not None and o.dval > 16:
                    s = dmasem[o.dma_key]
                    if o.dval - 16 > waited.get(id(s), 0) and o.dval - 16 > need.get(id(s), (None, 0))[1]:
                        need[id(s)] = (s, o.dval - 16)
                for k, (s, v) in need.items():
                    h.wait_ge(s, v)
                    waited[k] = v
                if o.fn is None:
                    continue
                ins = o.fn()
                if o.dma_key is not None:
                    ins.then_inc(dmasem[o.dma_key], 16)
                elif o.signal:
                    s, _ = semval(o)
                    ins.then_inc(s, 1)

        block.tensor(lambda eng: run("pe"))
        block.scalar(lambda eng: run("act"))
        block.vector(lambda eng: run("dve"))
        block.gpsimd(lambda eng: run("pool"))
        block.sync(lambda eng: run("sp"))


class Rot:
    def __init__(self, items):
        self.items = items
        self.i = 0

    def next(self):
        x = self.items[self.i % len(self.items)]
        self.i += 1
        return x


def host_consts():
    j = np.arange(128)[:, None]
    i = np.arange(128)[None, :]
    same = (j // 64) == (i // 64)
    ident = (j == i).astype(np.float32)
    tri = (same & (j <= i)).astype(np.float32)
    triu = (same & (j > i)).astype(np.float32)
    ones = np.ones((128, 128), np.float32)
    cmask = np.where(j <= i, 0.0, NEG).astype(np.float32)
    ind = np.zeros((128, 8, 128), np.float32)
    for b in range(8):
        ind[b, b, :] = 1.0
    cf = np.concatenate([ident, tri, triu], axis=1)
    cb = np.concatenate([ident, ones, cmask, ind.reshape(128, 1024)], axis=1)
    return np.ascontiguousarray(cf), np.ascontiguousarray(cb)


NCF, NCB = 384, 1408
V_NM, V_NF, V_OG, V_QG, V_KG = 0, 16, 32, 34, 35
NV = 36
NA = 70400


def build(stage=4):
    nc = bass.Bass("TRN2", target_bir_lowering=False)
    dt = nc.dram_tensor
    x_d = dt("x", [S, D], F32, kind="ExternalInput").ap()
    cf_d = dt("constsf", [128, NCF], F32, kind="ExternalInput").ap()
    cb_d = dt("constsb", [128, NCB], F32, kind="ExternalInput").ap()
    vecs_d = dt("vecs", [128, NV], F32, kind="ExternalInput").ap()
    gla_w_in = dt("gla_w_in", [D, 3088], F32, kind="ExternalInput").ap()
    gla_w_gate2 = dt("gla_w_gate2", [16, 512], F32, kind="ExternalInput").ap()
    gla_b_gate = dt("gla_b_gate", [1, 512], F32, kind="ExternalInput").ap()
    gla_w_out = dt("gla_w_out", [D, D], F32, kind="ExternalInput").ap()
    moba_w_qkv = dt("moba_w_qkv", [D, 3072], F32, kind="ExternalInput").ap()
    moba_w_out = dt("moba_w_out", [D, D], F32, kind="ExternalInput").ap()
    ffn_w_gate = dt("ffn_w_gate", [D, DFF], F32, kind="ExternalInput").ap()
    ffn_w_up = dt("ffn_w_up", [D, DFF], F32, kind="ExternalInput").ap()
    ffn_w_down = dt("ffn_w_down", [DFF, D], F32, kind="ExternalInput").ap()
    moe_w_router = dt("moe_w_router", [D, NE], F32, kind="ExternalInput").ap()
    moe_w_gate = dt("moe_w_gate", [NE, D, DFF], F32, kind="ExternalInput").ap()
    moe_w_up = dt("moe_w_up", [NE, D, DFF], F32, kind="ExternalInput").ap()
    moe_w_down = dt("moe_w_down", [NE, DFF, D], F32, kind="ExternalInput").ap()
    out_d = dt("out", [S, D], F32, kind="ExternalOutput").ap()

    sc = Sched()
    PE, ACT, DVE, POOL, SP = "pe", "act", "dve", "pool", "sp"
    te, se, ve, ge, sy = nc.tensor, nc.scalar, nc.vector, nc.gpsimd, nc.sync

    with ExitStack() as es:
        def sb(name, shape, dtype):
            return es.enter_context(nc.sbuf_tensor("sb_" + name, shape, dtype))

        xT = sb("xT", [128, NCH, S], F32)
        xT_b = [[Buf(f"xT{c}_{t}") for t in range(16)] for c in range(NCH)]
        cF = sb("cF", [128, NCF], F32)
        cB = sb("cB", [128, NCB], BF16)
        cF_b, cB_b = Buf("cF"), Buf("cB")
        vecs = sb("vecs", [128, NV], F32)
        vecs_b = Buf("vecs")
        arena = sb("arena", [128, NA], BF16)
        banks = [es.enter_context(nc.psum_tensor(f"bank{i}", [128, 512], F32)) for i in range(8)]
        bank_b = [[b_, b_] for b_ in [Buf(f"bank{i}") for i in range(8)]]

        class Arena:
            def __init__(self):
                self.off = 0

            def reset(self):
                self.off = 0

            def _shape(self, v, shape):
                if len(shape) == 2:
                    return v.rearrange("p (a b) -> p a b", a=shape[0])
                if len(shape) == 3:
                    return v.rearrange("p (a b c) -> p a b c", a=shape[0], b=shape[1])
                return v

            def b(self, *shape, parts=128):
                n = int(np.prod(shape))
                assert self.off + n <= NA, (self.off, n)
                v = arena[0:parts, self.off:self.off + n]
                self.off += n
                return self._shape(v, shape)

            def f(self, *shape, parts=128):
                n = int(np.prod(shape))
                self.off += self.off % 2
                assert self.off + 2 * n <= NA, (self.off, n)
                v = arena[0:parts, self.off:self.off + 2 * n].bitcast(F32)
                self.off += 2 * n
                return self._shape(v, shape)

        A = Arena()
        ident_f = cF[:, 0:128]
        tri_f = cF[:, 128:256]
        triu_f = cF[:, 256:384]
        ident_b = cB[:, 0:128]
        ones_b = cB[:, 128:256]
        cmask_b = cB[:, 256:384]

        def ind_b(kb):
            return cB[0:8, 384 + kb * 128:384 + (kb + 1) * 128]

        def ind_full(kb):
            return cB[:, 384 + kb * 128:384 + (kb + 1) * 128]

        def xbufs(cs, t0, t1):
            return [xT_b[c][t] for c in cs for t in range(t0, t1)]

        def bk(i):
            return bank_b[i]

        sc.op(SP, lambda: sy.dma_start(out=cF[:], in_=cf_d[:, :]), writes=[cF_b], dma_key="cF")
        sc.op(POOL, lambda: ge.dma_start(out=cB[:], in_=cb_d[:, :]), writes=[cB_b], dma_key="cB")
        sc.op(SP, lambda: sy.dma_start(out=vecs[:], in_=vecs_d[:, :]), writes=[vecs_b], dma_key="vecs")

        A.off = NA - 4096
        xin = [A.f(1024) for i in range(2)]
        xin_b = [Buf("xin0"), Buf("xin1")]
        for t in range(16):
            s = t % 2
            sc.op(SP, (lambda s=s, t=t: sy.dma_start(out=xin[s], in_=x_d[t * 128:(t + 1) * 128, :])),
                  writes=[xin_b[s]], dma_key=f"xin{s}")
            for half in range(2):
                b = 2 * (t % 2) + half
                for q in range(4):
                    c = half * 4 + q
                    sc.op(PE, (lambda b=b, q=q, c=c, s=s: te.transpose(banks[b][:, q * 128:(q + 1) * 128],
                                                                      xin[s][:, c * 128:(c + 1) * 128], ident_f)),
                          reads=[xin_b[s], cF_b], writes=bk(b))
                if half == 0:
                    fn = (lambda b=b, t=t: se.copy(out=xT[:, 0:4, t * 128:(t + 1) * 128],
                                                   in_=banks[b][:].rearrange("p (a b) -> p a b", a=4)))
                    sc.op(ACT, fn, reads=bk(b), writes=xbufs(range(0, 4), t, t + 1))
                else:
                    fn = (lambda b=b, t=t: ve.tensor_copy(out=xT[:, 4:8, t * 128:(t + 1) * 128],
                                                          in_=banks[b][:].rearrange("p (a b) -> p a b", a=4)))
                    sc.op(DVE, fn, reads=bk(b), writes=xbufs(range(4, 8), t, t + 1))

        def load_w(dst, dst_b, src, key):
            sc.op(POOL, lambda: ge.dma_start(out=dst, in_=src), writes=[dst_b], dma_key=key)

        def rms_rstd(src_fn, src_bufs, nchunks, n, sq, sq_b, ssbank, rs_t, rs_b, inv_n):
            for c in range(nchunks):
                s = c % 2
                sc.op(ACT, (lambda c=c, s=s: se.activation(out=sq[s], in_=src_fn(c), func=AF.Square)),
                      reads=src_bufs(c), writes=[sq_b[s]])
                sc.op(PE, (lambda c=c, s=s: te.matmul(banks[ssbank][:, 0:n], lhsT=ones_b, rhs=sq[s],
                                                      start=(c == 0), stop=(c == nchunks - 1))),
                      reads=[sq_b[s], cB_b], writes=bk(ssbank))
            sc.op(ACT, lambda: se.activation(out=rs_t, in_=banks[ssbank][:, 0:n], func=AF.Ln, bias=EPS, scale=inv_n),
                  reads=bk(ssbank), writes=[rs_b])
            sc.op(ACT, lambda: se.activation(out=rs_t, in_=rs_t, func=AF.Exp, scale=-0.5), reads=[rs_b], writes=[rs_b])

        def norm_to(n, t0, gcol, h_fn, h_bufs, sq, sq_b, ssbank, rs_t, rs_b):
            nt = n // 128
            c0 = t0 * 128
            rms_rstd(lambda c: xT[:, c, c0:c0 + n], lambda c: xbufs([c], t0, t0 + nt), 8, n, sq, sq_b, ssbank, rs_t, rs_b, 1.0 / D)
            for c in range(8):
                sc.op(DVE, (lambda c=c: ve.scalar_tensor_tensor(out=h_fn(c), in0=xT[:, c, c0:c0 + n],
                                                                scalar=vecs[:, gcol + c:gcol + c + 1], in1=rs_t,
                                                                op0=ALU.mult, op1=ALU.mult)),
                      reads=xbufs([c], t0, t0 + nt) + [rs_b, vecs_b], writes=h_bufs(c))

        def add_to_x(bank, n, dc, t0):
            nt = n // 128
            c0 = t0 * 128
            sc.op(DVE, lambda: ve.tensor_tensor(out=xT[:, dc, c0:c0 + n], in0=xT[:, dc, c0:c0 + n],
                                                in1=banks[bank][:, 0:n], op=ALU.add),
                  reads=bk(bank) + xbufs([dc], t0, t0 + nt), writes=xbufs([dc], t0, t0 + nt))

        def gla_phase():
            A.reset()
            GN = 256
            Win = A.b(8, 3088)
            Wout = A.b(8, 1024)
            hTg = A.b(8, GN)
            sq = [A.b(GN) for _ in range(2)]
            qdec = A.b(4, GN)
            kinv = A.b(4, GN)
            kend = [A.b(512) for _ in range(2)]
            Vt = [A.b(1024) for _ in range(2)]
            onT = A.b(8, GN)
            STm = [A.b(128) for _ in range(2)]
            stbf = [A.b(256) for _ in range(4)]
            osq = [A.b(2, 128) for _ in range(4)]
            sg2 = [A.b(8, GN), A.b(8, GN)]
            E1 = A.f(4, GN)
            E2 = A.f(4, GN)
            E3 = [A.f(512) for _ in range(2)]
            Lt = [A.f(512) for _ in range(2)]
            tmpE = A.f(512)
            stf = [A.f(256) for _ in range(4)]
            rsg = A.f(GN)
            ors4 = A.f(512)
            ot1 = [A.f(128) for _ in range(2)]
            a_aug = A.f(GN, parts=17)
            W2aug = A.f(512, parts=17)
            Win_b = [Buf(f"Win{k}") for k in range(8)]
            Wout_b, hTg_b, qdec_b, kinv_b = Buf("Wout"), Buf("hTg"), Buf("qdec"), Buf("kinv")
            sq_b = [Buf("sq0"), Buf("sq1")]
            kend_b = [Buf("kend0"), Buf("kend1")]
            Vt_b = [Buf("Vt0"), Buf("Vt1")]
            onT_b = [Buf(f"onT{t}") for t in range(2)]
            STm_b = [Buf("STm0"), Buf("STm1")]
            stbf_b = [Buf(f"stbf{i}") for i in range(4)]
            stf_b = [Buf(f"stf{i}") for i in range(4)]
            osq_b = [Buf(f"osq{i}") for i in range(4)]
            ors4_b = Buf("ors4")
            E1_b = [Buf(f"E1_{t}") for t in range(2)]
            E2_b = [Buf(f"E2_{t}") for t in range(2)]
            rsg_b = Buf("rsg")
            sg2_b = [Buf("sg0"), Buf("sg1")]
            ors_b = [Buf("ors0"), Buf("ors1")]
            ot1_b = [Buf("ot10"), Buf("ot11")]
            Lt_b = [Buf("L0"), Buf("L1")]
            tmpE_b = Buf("tmpE")
            E3_b = [Buf("E30"), Buf("E31")]
            aaug_b, W2_b = Buf("aaug"), Buf("W2aug")

            for k in range(8):
                load_w(Win[:, k, :], Win_b[k], gla_w_in[k * 128:(k + 1) * 128, :], f"Win{k}")
            load_w(Wout, Wout_b, gla_w_out.rearrange("(k p) n -> p k n", p=128), "Wout")
            sc.barrier()
            sc.op(SP, lambda: sy.dma_start(out=W2aug[0:16, :], in_=gla_w_gate2[:, :]), writes=[W2_b], dma_key="W2")
            sc.op(SP, lambda: sy.dma_start(out=W2aug[16:17, :], in_=gla_b_gate[:, :]), writes=[W2_b], dma_key="W2")
            sc.op(POOL, lambda: ge.memset(a_aug, 1.0), writes=[aaug_b])
            for h in range(4):
                sc.op(POOL, (lambda h=h: ge.memset(stf[h], 0.0)), writes=[stf_b[h]])
                sc.op(POOL, (lambda h=h: ge.memset(stbf[h], 0.0)), writes=[stbf_b[h]])

            rA = Rot([0, 1])
            rB = Rot([2, 3])
            rC = Rot([4, 5])
            rF = Rot([2, 3, 4, 5])
            def prenorm1a(g):
                T0 = g * 2
                c0 = T0 * 128
                rms_rstd(lambda c: xT[:, c, c0:c0 + GN], lambda c: xbufs([c], T0, T0 + 2), 8, GN, sq, sq_b, rA.next(), rsg, rsg_b, 1.0 / D)

            def prenorm1b(g, hTg, hTg_b):
                T0 = g * 2
                c0 = T0 * 128
                for c in range(8):
                    sc.op(DVE, (lambda c=c: ve.scalar_tensor_tensor(out=hTg[:, c, :], in0=xT[:, c, c0:c0 + GN],
                                                                    scalar=vecs[:, V_NM + c:V_NM + c + 1], in1=rsg,
                                                                    op0=ALU.mult, op1=ALU.mult)),
                          reads=xbufs([c], T0, T0 + 2) + [rsg_b, vecs_b], writes=[hTg_b])

            def prenorm2(g, hTg, hTg_b):
                ba = rA.next()
                for k in range(8):
                    sc.op(PE, (lambda k=k, ba=ba: te.matmul(banks[ba][0:16, 0:GN], lhsT=Win[:, k, 3072:3088], rhs=hTg[:, k, :],
                                                            start=(k == 0), stop=(k == 7))),
                          reads=[Win_b[k], hTg_b], writes=bk(ba))
                sc.op(ACT, (lambda ba=ba: se.copy(out=a_aug[0:16, :], in_=banks[ba][0:16, 0:GN])), reads=bk(ba), writes=[aaug_b])

            def do_group(g, hTg, hTg_b):
                T0 = g * 2
                sg, sg_b = sg2[g % 2], sg2_b[g % 2]

                def g_chunk(c, rot=None):
                    bq = (rot or rF).next()
                    for k in range(8):
                        sc.op(PE, (lambda bq=bq, k=k, c=c: te.matmul(banks[bq][:, 0:GN], lhsT=Win[:, k, 2048 + c * 128:2048 + (c + 1) * 128],
                                                                     rhs=hTg[:, k, :], start=(k == 0), stop=(k == 7))),
                              reads=[Win_b[k], hTg_b], writes=bk(bq))
                    sc.op(ACT, (lambda bq=bq, c=c: se.activation(out=sg[:, c, :], in_=banks[bq][:, 0:GN], func=AF.Silu)),
                          reads=bk(bq), writes=[sg_b])

                def z_part(t):
                    tc0 = t * 128
                    bz = rA.next()
                    sc.op(PE, (lambda bz=bz, tc0=tc0: te.matmul(banks[bz][:, :], lhsT=a_aug[0:17, tc0:tc0 + 128], rhs=W2aug[0:17, :],
                                                                start=True, stop=True)),
                          reads=[aaug_b, W2_b], writes=bk(bz))
                    sc.op(ACT, (lambda bz=bz: se.activation(out=tmpE, in_=banks[bz][:, :], func=AF.Exp, scale=-1.0)),
                          reads=bk(bz), writes=[tmpE_b])
                    sc.op(ACT, (lambda t=t: se.activation(out=Lt[t], in_=tmpE, func=AF.Ln, bias=1.0, scale=1.0)),
                          reads=[tmpE_b], writes=[Lt_b[t]])

                def cl_rl(t):
                    tc0 = t * 128
                    bc = rF.next()
                    for h in range(4):
                        sc.op(PE, (lambda bc=bc, h=h, t=t: te.matmul(banks[bc][:, h * 128:(h + 1) * 128],
                                                                     lhsT=Lt[t][:, h * 128:(h + 1) * 128], rhs=tri_f,
                                                                     start=True, stop=True)),
                              reads=[Lt_b[t], cF_b], writes=[bk(bc)[h // 2]])
                    sc.op(ACT, (lambda bc=bc, tc0=tc0: se.activation(out=E1[:, :, tc0:tc0 + 128],
                                                                     in_=banks[bc][:].rearrange("p (a b) -> p a b", a=4),
                                                                     func=AF.Exp, scale=-1.0 / 16.0)),
                          reads=bk(bc), writes=[E1_b[t]])
                    sc.op(ACT, (lambda bc=bc, tc0=tc0: se.activation(out=E2[:, :, tc0:tc0 + 128],
                                                                     in_=banks[bc][:].rearrange("p (a b) -> p a b", a=4),
                                                                     func=AF.Exp, scale=1.0 / 16.0)),
                          reads=bk(bc), writes=[E2_b[t]])
                    br = rA.next()
                    sc.op(PE, (lambda br=br, t=t: te.matmul(banks[br][:, :], lhsT=triu_f, rhs=Lt[t], start=True, stop=True)),
                          reads=[Lt_b[t], cF_b], writes=bk(br))
                    sc.op(ACT, (lambda br=br, t=t: se.activation(out=E3[t], in_=banks[br][:, :], func=AF.Exp, scale=-1.0 / 16.0)),
                          reads=bk(br), writes=[E3_b[t]])

                def kv_tok_v(t):
                    kv_tok(t, (("v0", 1024), ("v1", 1536)))

                def kv_tok_k(t):
                    kv_tok(t, (("k", 512),))

                def kv_tok(t, kinds):
                    tc0 = t * 128
                    for (kind, col0) in kinds:
                        bb = rF.next()
                        for k in range(8):
                            sc.op(PE, (lambda bb=bb, k=k, col0=col0, tc0=tc0: te.matmul(banks[bb][:, :], lhsT=hTg[:, k, tc0:tc0 + 128],
                                                                                        rhs=Win[:, k, col0:col0 + 512],
                                                                                        start=(k == 0), stop=(k == 7))),
                                  reads=[Win_b[k], hTg_b], writes=bk(bb))
                        if kind == "k":
                            sc.op(DVE, (lambda bb=bb, t=t: ve.tensor_tensor(out=kend[t], in0=banks[bb][:, :], in1=E3[t], op=ALU.mult)),
                                  reads=bk(bb) + [E3_b[t]], writes=[kend_b[t]])
                        elif kind == "v0":
                            sc.op(ACT, (lambda bb=bb, t=t: se.copy(out=Vt[t][:, 0:512], in_=banks[bb][:, :])),
                                  reads=bk(bb), writes=[Vt_b[t]])
                        else:
                            sc.op(ACT, (lambda bb=bb, t=t: se.copy(out=Vt[t][:, 512:1024], in_=banks[bb][:, :])),
                                  reads=bk(bb), writes=[Vt_b[t]])

                def qk_head(h):
                    bq = rF.next()
                    for k in range(8):
                        sc.op(PE, (lambda bq=bq, k=k, h=h: te.matmul(banks[bq][:, 0:GN], lhsT=Win[:, k, h * 128:(h + 1) * 128], rhs=hTg[:, k, :],
                                                                     start=(k == 0), stop=(k == 7))),
                              reads=[Win_b[k], hTg_b], writes=bk(bq))
                    sc.op(DVE, (lambda bq=bq, h=h: ve.scalar_tensor_tensor(out=qdec[:, h, :], in0=banks[bq][:, 0:GN], scalar=128.0 ** -0.5,
                                                                           in1=E1[:, h, :], op0=ALU.mult, op1=ALU.mult)),
                          reads=bk(bq) + E1_b, writes=[qdec_b])
                    bq = rF.next()
                    for k in range(8):
                        sc.op(PE, (lambda bq=bq, k=k, h=h: te.matmul(banks[bq][:, 0:GN], lhsT=Win[:, k, 512 + h * 128:512 + (h + 1) * 128],
                                                                     rhs=hTg[:, k, :], start=(k == 0), stop=(k == 7))),
                              reads=[Win_b[k], hTg_b], writes=bk(bq))
                    sc.op(DVE, (lambda bq=bq, h=h: ve.tensor_tensor(out=kinv[:, h, :], in0=banks[bq][:, 0:GN], in1=E2[:, h, :], op=ALU.mult)),
                          reads=bk(bq) + E2_b, writes=[kinv_b])

                def F1():
                    for c in range(8):
                        g_chunk(c, rot=rA)

                def F2():
                    z_part(0); z_part(1)
                    kv_tok_v(0)
                    cl_rl(0); cl_rl(1)
                    kv_tok_v(1)
                    kv_tok_k(0); kv_tok_k(1)
                    for h in range(4):
                        qk_head(h)

                def R(t):
                    tc0 = t * 128
                    obase = 6 if t == 0 else 2
                    for h in range(4):
                        half = h % 2
                        sm = h % 2
                        b5 = rC.next()
                        sc.op(PE, (lambda h=h, b5=b5, tc0=tc0: te.matmul(banks[b5][:, 0:128],
                                                                         lhsT=kinv[:, h, tc0:tc0 + 128], rhs=qdec[:, h, tc0:tc0 + 128],
                                                                         start=True, stop=True)),
                              reads=[kinv_b, qdec_b], writes=bk(b5))
                        sc.op(DVE, (lambda b5=b5, sm=sm: ve.tensor_tensor(out=STm[sm], in0=banks[b5][:, 0:128],
                                                                          in1=tri_f, op=ALU.mult)),
                              reads=bk(b5) + [cF_b], writes=[STm_b[sm]])
                        for vc in range(2):
                            ob = obase + h // 2
                            oc = ((h % 2) * 2 + vc) * 128
                            sc.op(PE, (lambda ob=ob, oc=oc, t=t, h=h, vc=vc, sm=sm: te.matmul(
                                banks[ob][:, oc:oc + 128], lhsT=Vt[t][:, h * 256 + vc * 128:h * 256 + (vc + 1) * 128], rhs=STm[sm],
                                start=(h % 2 == 0 and vc == 0), stop=False)), reads=[Vt_b[t], STm_b[sm]], writes=[bk(ob)[h % 2]])
                    for cc in range(2):
                        cs = tc0 + cc * 64
                        for h in range(4):
                            for vc in range(2):
                                ob = obase + h // 2
                                oc = ((h % 2) * 2 + vc) * 128 + cc * 64
                                sc.op(PE, (lambda ob=ob, oc=oc, h=h, vc=vc, cs=cs, cc=cc: te.matmul(
                                    banks[ob][:, oc:oc + 64], lhsT=stbf[h][:, vc * 128:(vc + 1) * 128], rhs=qdec[:, h, cs:cs + 64],
                                    start=False, stop=(cc == 1 and h % 2 == 1 and vc == 1))), reads=[stbf_b[h], qdec_b], writes=[bk(ob)[h % 2]])
                        for h in range(4):
                            half = h % 2
                            b5 = rC.next()
                            sc.op(PE, (lambda h=h, b5=b5, t=t, cc=cc: te.matmul(
                                banks[b5][:, 0:256], lhsT=kend[t][cc * 64:(cc + 1) * 64, h * 128:(h + 1) * 128],
                                rhs=Vt[t][cc * 64:(cc + 1) * 64, h * 256:(h + 1) * 256], start=True, stop=True)),
                                reads=[kend_b[t], Vt_b[t]], writes=bk(b5))
                            sc.op(DVE, (lambda h=h, b5=b5, cs=cs: ve.scalar_tensor_tensor(
                                out=stf[h], in0=stf[h], scalar=E1[:, h, cs + 63:cs + 64], in1=banks[b5][:, 0:256],
                                op0=ALU.mult, op1=ALU.add)), reads=[stf_b[h], E1_b[t]] + bk(b5), writes=[stf_b[h]])
                            sc.op(ACT, (lambda h=h: se.copy(out=stbf[h], in_=stf[h])), reads=[stf_b[h]], writes=[stbf_b[h]])
                def ON_s(t):
                    obase = 6 if t == 0 else 2
                    bs = rA.next()
                    for h in range(4):
                        ob = obase + h // 2
                        oc = (h % 2) * 256
                        sc.op(ACT, (lambda ob=ob, oc=oc, h=h: se.activation(out=osq[h],
                                                                            in_=banks[ob][:, oc:oc + 256].rearrange("p (a b) -> p a b", a=2),
                                                                            func=AF.Square)),
                              reads=[bk(ob)[h % 2]], writes=[osq_b[h]])
                    for h in range(4):
                        for vc in range(2):
                            sc.op(PE, (lambda bs=bs, vc=vc, h=h: te.matmul(banks[bs][:, h * 128:(h + 1) * 128], lhsT=ones_b, rhs=osq[h][:, vc, :],
                                                                           start=(h == 0 and vc == 0), stop=(h == 3 and vc == 1))),
                                  reads=[osq_b[h], cB_b], writes=bk(bs))
                    sc.op(ACT, (lambda bs=bs: se.activation(out=ors4, in_=banks[bs][:, :], func=AF.Ln, bias=EPS, scale=1.0 / 256.0)),
                          reads=bk(bs), writes=[ors4_b])
                    sc.op(ACT, lambda: se.activation(out=ors4, in_=ors4, func=AF.Exp, scale=-0.5), reads=[ors4_b], writes=[ors4_b])

                def ON_a(t):
                    obase = 6 if t == 0 else 2
                    tc0 = t * 128
                    for h in range(4):
                        ob = obase + h // 2
                        oc = (h % 2) * 256
                        for vc in range(2):
                            c = h * 2 + vc
                            s1 = vc
                            sc.op(DVE, (lambda ob=ob, oc=oc, vc=vc, s1=s1, h=h: ve.scalar_tensor_tensor(
                                out=ot1[s1], in0=banks[ob][:, oc + vc * 128:oc + (vc + 1) * 128], scalar=vecs[:, V_OG + vc:V_OG + vc + 1],
                                in1=ors4[:, h * 128:(h + 1) * 128], op0=ALU.mult, op1=ALU.mult)),
                                reads=[bk(ob)[h % 2], ors4_b, vecs_b], writes=[ot1_b[s1]])
                            sc.op(DVE, (lambda c=c, s1=s1, tc0=tc0: ve.tensor_tensor(out=onT[:, c, tc0:tc0 + 128], in0=ot1[s1],
                                                                                     in1=sg[:, c, tc0:tc0 + 128], op=ALU.mult)),
                                  reads=[ot1_b[s1], sg_b], writes=[onT_b[t]])
                def O():
                    for dc in range(8):
                        bo = rC.next()
                        for c in range(8):
                            sc.op(PE, (lambda bo=bo, c=c, dc=dc: te.matmul(banks[bo][:, 0:GN], lhsT=Wout[:, c, dc * 128:(dc + 1) * 128], rhs=onT[:, c, :],
                                                                           start=(c == 0), stop=(c == 7))),
                                  reads=[Wout_b] + onT_b, writes=bk(bo))
                        add_to_x(bo, GN, dc, T0)

                return F1, F2, R, ON_s, ON_a, O

            hT2 = [hTg, A.b(8, GN)]
            hT2_b = [hTg_b, Buf("hTg1")]
            NG = S // GN
            stages = [do_group(g, hT2[g % 2], hT2_b[g % 2]) for g in range(NG)]
            prenorm1a(0)
            prenorm1b(0, hT2[0], hT2_b[0])
            prenorm2(0, hT2[0], hT2_b[0])
            stages[0][0]()
            stages[0][1]()
            for g in range(NG):
                F1, F2, R, ON_s, ON_a, O = stages[g]
                nx = g + 1 < NG
                nh, nhb = hT2[(g + 1) % 2], hT2_b[(g + 1) % 2]
                if nx:
                    prenorm1a(g + 1)
                R(0)
                ON_s(0)
                if nx:
                    prenorm1b(g + 1, nh, nhb)
                    prenorm2(g + 1, nh, nhb)
                R(1)
                ON_a(0)
                ON_s(1)
                ON_a(1)
                if nx:
                    stages[g + 1][0]()
                O()
                if nx:
                    stages[g + 1][1]()

        class FFN:
            def __init__(self, with_gate):
                self.hT = A.b(8, S)
                self.hT_b = [[Buf(f"hT{c}_{g}") for g in range(4)] for c in range(8)]
                self.act = A.b(4, S)
                self.act_b = [[Buf(f"act{f}_{g}") for g in range(4)] for f in range(4)]
                self.Wg = [A.b(8, 512) for _ in range(2)]
                self.Wu = [A.b(8, 512) for _ in range(2)]
                self.Wd = [A.b(4, 1024) for _ in range(2)]
                self.Wg_b = [Buf("Wg0"), Buf("Wg1")]
                self.Wu_b = [Buf("Wu0"), Buf("Wu1")]
                self.Wd_b = [Buf("Wd0"), Buf("Wd1")]
                self.sil = [A.f(512) for _ in range(3)]
                self.sil_b = [Buf(f"sil{i}") for i in range(3)]
                self.rsil = Rot([0, 1, 2])
                if with_gate:
                    self.sil2 = [A.f(512) for _ in range(2)]
                    self.sil2_b = [Buf(f"sil2{i}") for i in range(2)]
                    self.rsil2 = Rot([0, 1])
                self.rGU = Rot([0, 1, 2, 3, 4])
                self.rD = Rot([5, 6, 7])
                self.cnt = 0

            def run(self, wg, wu, wd, gbc=None, gbc_b=None, mid_hook=None):
                hT, act = self.hT, self.act
                for scn in range(7):
                    s = self.cnt % 2
                    self.cnt += 1
                    load_w(self.Wg[s], self.Wg_b[s], wg[:, scn * 512:(scn + 1) * 512].rearrange("(k p) n -> p k n", p=128), f"Wg{s}")
                    load_w(self.Wu[s], self.Wu_b[s], wu[:, scn * 512:(scn + 1) * 512].rearrange("(k p) n -> p k n", p=128), f"Wu{s}")
                    load_w(self.Wd[s], self.Wd_b[s], wd[scn * 512:(scn + 1) * 512, :].rearrange("(f p) n -> p f n", p=128), f"Wd{s}")
                    Wg, Wu, Wd = self.Wg[s], self.Wu[s], self.Wd[s]
                    for f in range(4):
                        for tg in range(4):
                            c0 = tg * 512
                            pg, pu = self.rGU.next(), self.rGU.next()
                            for (pb, W, Wb) in ((pg, Wg, self.Wg_b[s]), (pu, Wu, self.Wu_b[s])):
                                for k in range(8):
                                    sc.op(PE, (lambda pb=pb, W=W, k=k, f=f, c0=c0: te.matmul(
                                        banks[pb][:, :], lhsT=W[:, k, f * 128:(f + 1) * 128], rhs=hT[:, k, c0:c0 + 512],
                                        start=(k == 0), stop=(k == 7))), reads=[Wb, self.hT_b[k][tg]], writes=bk(pb))
                            si = self.rsil.next()
                            sc.op(ACT, (lambda pg=pg, si=si: se.activation(out=self.sil[si], in_=banks[pg][:, :], func=AF.Silu)),
                                  reads=bk(pg), writes=[self.sil_b[si]])
                            src, src_b = self.sil[si], self.sil_b[si]
                            if gbc is not None:
                                s2 = self.rsil2.next()
                                sc.op(POOL, (lambda si=si, s2=s2, c0=c0: ge.tensor_tensor(out=self.sil2[s2], in0=self.sil[si],
                                                                                          in1=gbc[:, c0:c0 + 512], op=ALU.mult)),
                                      reads=[self.sil_b[si], gbc_b[tg]], writes=[self.sil2_b[s2]])
                                src, src_b = self.sil2[s2], self.sil2_b[s2]
                            sc.op(DVE, (lambda src=src, pu=pu, f=f, c0=c0: ve.tensor_tensor(out=act[:, f, c0:c0 + 512], in0=src,
                                                                                            in1=banks[pu][:, :], op=ALU.mult)),
                                  reads=[src_b] + bk(pu), writes=[self.act_b[f][tg]])
                    if mid_hook is not None and scn == 3:
                        mid_hook()
                    for tg in range(4):
                        for dc in range(8):
                            c0 = tg * 512
                            pd = self.rD.next()
                            for f in range(4):
                                sc.op(PE, (lambda pd=pd, f=f, dc=dc, c0=c0, Wd=Wd: te.matmul(
                                    banks[pd][:, :], lhsT=Wd[:, f, dc * 128:(dc + 1) * 128], rhs=act[:, f, c0:c0 + 512],
                                    start=(f == 0), stop=(f == 3))), reads=[self.Wd_b[s], self.act_b[f][tg]], writes=bk(pd))
                            add_to_x(pd, 512, dc, tg * 4)

        def ffn0_phase():
            sc.barrier()
            A.reset()
            F = FFN(False)
            sq = [A.b(512) for _ in range(2)]
            sq_b = [Buf("sq0"), Buf("sq1")]
            rs = A.f(512)
            rs_b = Buf("rs")
            for g in range(4):
                norm_to(512, g * 4, V_NF + 0, lambda c, g=g: F.hT[:, c, g * 512:(g + 1) * 512], lambda c, g=g: [F.hT_b[c][g]],
                        sq, sq_b, 6 + g % 2, rs, rs_b)
            F.run(ffn_w_gate, ffn_w_up, ffn_w_down)

        def moba_phase():
            sc.barrier()
            A.reset()
            kT = A.b(8, S)
            Vv = A.b(16, 1024)
            Wkv = A.b(8, 2048)
            hTg = A.b(8, 512)
            sq = [A.b(512) for _ in range(2)]
            qn = A.b(8, 256)
            onT = A.b(8, 256)
            NP = 4
            Pb = [A.b(256) for _ in range(NP)]
            nselT = A.b(8, 256)
            rs2 = [A.f(512) for _ in range(2)]
            knf = [A.f(512) for _ in range(2)]
            ksum = A.f(8, 8)
            qnf = [knf[i][:, 0:256] for i in range(2)]
            gsb = A.f(16, 8)
            cmpb = A.f(16, 8, 8)
            rank = A.f(16, 8)
            nsel = A.f(16, 8)
            rden = [A.f(256) for _ in range(2)]
            kT_b = [[Buf(f"kT{h}_{g}") for g in range(4)] for h in range(8)]
            Vv_b = [Buf(f"V{t}") for t in range(16)]
            Wk_b = [Buf(f"Wkv{k}") for k in range(8)]
            hTg_b = Buf("hTg")
            sq_b = [Buf("sq0"), Buf("sq1")]
            qn_b = [Buf(f"qn{h}") for h in range(8)]
            onT_b = [Buf(f"onT{h}") for h in range(8)]
            Pb_b = [Buf(f"P{i}") for i in range(NP)]
            nselT_b = [Buf(f"nselT{h}") for h in range(8)]
            rs2_b = [Buf("rs0"), Buf("rs1")]
            knf_b = [Buf("knf0"), Buf("knf1")]
            ksum_b = Buf("ksum")
            qnf_b = knf_b
            gsb_b, cmpb_b, rank_b, nsel_b = Buf("gsb"), Buf("cmpb"), Buf("rank"), Buf("nsel")
            rden_b = [Buf("rden0"), Buf("rden1")]
            rA = Rot([0, 1])
            rB = Rot([2, 3, 4])
            for k in range(8):
                load_w(Wkv[:, k, :], Wk_b[k], moba_w_qkv[k * 128:(k + 1) * 128, 1024:3072], f"Wkv{k}")
            sc.op(POOL, lambda: ge.memset(nselT, 0.0), writes=nselT_b)

            rB4 = Rot([2, 3, 4, 5])

            def head_norm_pipeline(nheads, n, proj_mm, gain_col, finish, rot=None, filler=None):
                rot = rot or rB
                pend = None
                for h in range(nheads + 1):
                    cur = None
                    if h < nheads:
                        pk = rot.next()
                        proj_mm(h, pk)
                        s = h % 2
                        sc.op(ACT, (lambda pk=pk, s=s: se.activation(out=sq[s][:, 0:n], in_=banks[pk][:, 0:n], func=AF.Square)),
                              reads=bk(pk), writes=[sq_b[s]])
                        cur = (h, pk, s)
                        if filler is not None:
                            filler(h)
                    if pend is not None:
                        ph, ppk, ps = pend
                        bs = rA.next()
                        sc.op(PE, (lambda bs=bs, ps=ps: te.matmul(banks[bs][:, 0:n], lhsT=ones_b, rhs=sq[ps][:, 0:n], start=True, stop=True)),
                              reads=[sq_b[ps], cB_b], writes=bk(bs))
                        sc.op(ACT, (lambda bs=bs, ps=ps: se.activation(out=rs2[ps][:, 0:n], in_=banks[bs][:, 0:n], func=AF.Ln,
                                                                        bias=EPS, scale=1.0 / 128.0)),
                              reads=bk(bs), writes=[rs2_b[ps]])
                        sc.op(ACT, (lambda ps=ps: se.activation(out=rs2[ps][:, 0:n], in_=rs2[ps][:, 0:n], func=AF.Exp, scale=-0.5)),
                              reads=[rs2_b[ps]], writes=[rs2_b[ps]])
                        finish(ph, ppk, ps)
                    pend = cur

            for g in range(4):
                norm_to(512, g * 4, V_NM + 8, lambda c: hTg[:, c, :], lambda c: [hTg_b], sq, sq_b, rA.next(), rs2[0], rs2_b[0])

                def k_proj(h, pk):
                    for k in range(8):
                        sc.op(PE, (lambda pk=pk, k=k, h=h: te.matmul(banks[pk][:, :], lhsT=Wkv[:, k, h * 128:(h + 1) * 128], rhs=hTg[:, k, :],
                                                                     start=(k == 0), stop=(k == 7))),
                              reads=[Wk_b[k], hTg_b], writes=bk(pk))

                def k_finish(h, pk, s, g=g):
                    sc.op(DVE, (lambda pk=pk, s=s: ve.scalar_tensor_tensor(out=knf[s], in0=banks[pk][:, :], scalar=vecs[:, V_KG:V_KG + 1],
                                                                           in1=rs2[s], op0=ALU.mult, op1=ALU.mult)),
                          reads=bk(pk) + [rs2_b[s], vecs_b], writes=[knf_b[s]])
                    sc.op(POOL, (lambda s=s, h=h, g=g: ge.tensor_copy(out=kT[:, h, g * 512:(g + 1) * 512], in_=knf[s])),
                          reads=[knf_b[s]], writes=[kT_b[h][g]])
                    sc.op(DVE, (lambda s=s, h=h, g=g: ve.tensor_reduce(out=ksum[:, h, 2 * g:2 * g + 2],
                                                                       in_=knf[s].rearrange("p (a b) -> p a b", a=2),
                                                                       axis=AX.X, op=ALU.add)),
                          reads=[knf_b[s]], writes=[ksum_b])

                def v_fill(h, g=g):
                    t, half = h // 2, h % 2
                    tt = g * 4 + t
                    pv = rA.next()
                    for k in range(8):
                        sc.op(PE, (lambda pv=pv, k=k, t=t, half=half: te.matmul(
                            banks[pv][:, :], lhsT=hTg[:, k, t * 128:(t + 1) * 128], rhs=Wkv[:, k, 1024 + half * 512:1024 + (half + 1) * 512],
                            start=(k == 0), stop=(k == 7))), reads=[Wk_b[k], hTg_b], writes=bk(pv))
                    sc.op(DVE, (lambda pv=pv, tt=tt, half=half: ve.tensor_copy(out=Vv[:, tt, half * 512:(half + 1) * 512], in_=banks[pv][:, :])),
                          reads=bk(pv), writes=[Vv_b[tt]])

                head_norm_pipeline(8, 512, k_proj, V_KG, k_finish, rot=rB4, filler=v_fill)
            Wq = Wkv[:, :, 0:1024]
            Wo = Wkv[:, :, 1024:2048]
            Wq_b = [Buf(f"Wq{k}") for k in range(8)]
            Wo_b = Buf("Wo")
            for k in range(8):
                sc.op(POOL, (lambda k=k: ge.dma_start(out=Wq[:, k, :], in_=moba_w_qkv[k * 128:(k + 1) * 128, 0:1024])),
                      reads=[], writes=[Wq_b[k], Wk_b[k]], dma_key=f"Wq{k}")
            sc.op(POOL, lambda: ge.dma_start(out=Wo, in_=moba_w_out.rearrange("(k p) n -> p k n", p=128)),
                  writes=[Wo_b] + Wk_b, dma_key="Wo")
            hTb2 = [hTg[:, :, 0:256], hTg[:, :, 256:512]]
            hTb2_b = [Buf("hTb0"), Buf("hTb1")]
            rS = Rot([2, 3, 4])
            rP = Rot(list(range(NP)))
            rQ = Rot([5, 1])
            LA = 2
            scale = 128.0 ** -0.5
            sq256 = [x[:, 0:256] for x in sq]

            def q_norm(b):
                norm_to(256, b * 2, V_NM + 8, lambda c: hTb2[b % 2][:, c, :], lambda c: [hTb2_b[b % 2], hTg_b], sq256, sq_b, 0,
                        rs2[0][:, 0:256], rs2_b[0])

            class QPipe:
                def __init__(self, b):
                    self.b = b
                    self.hT = hTb2[b % 2]
                    self.hT_b = hTb2_b[b % 2]
                    self.pend = None
                    self.gate_pend = None
                    self.h = 0

                def step(self):
                    b, h = self.b, self.h
                    cur = None
                    if self.gate_pend is not None:
                        gh, gs = self.gate_pend
                        for tt in range(2):
                            m = gh * 2 + tt
                            sc.op(PE, (lambda gs=gs, tt=tt, gh=gh, m=m: te.matmul(banks[0][:, 384 + m * 8:384 + (m + 1) * 8],
                                                                                  lhsT=qnf[gs][:, tt * 128:(tt + 1) * 128],
                                                                                  rhs=ksum[:, gh, :], start=True, stop=True)),
                                  reads=[qnf_b[gs], ksum_b], writes=bk(0))
                        self.gate_pend = None
                    if h < 8:
                        pq = rQ.next()
                        for k in range(8):
                            sc.op(PE, (lambda pq=pq, k=k, h=h, hT=self.hT: te.matmul(banks[pq][:, 0:256], lhsT=Wq[:, k, h * 128:(h + 1) * 128],
                                                                                   rhs=hT[:, k, :], start=(k == 0), stop=(k == 7))),
                                  reads=[Wq_b[k], self.hT_b], writes=bk(pq))
                        s = h % 2
                        sc.op(ACT, (lambda pq=pq, s=s: se.activation(out=sq256[s], in_=banks[pq][:, 0:256], func=AF.Square)),
                              reads=bk(pq), writes=[sq_b[s]])
                        cur = (h, pq, s)
                    if self.pend is not None:
                        ph, ppq, ps = self.pend
                        sc.op(PE, (lambda ps=ps: te.matmul(banks[0][:, 0:256], lhsT=ones_b, rhs=sq256[ps], start=True, stop=True)),
                              reads=[sq_b[ps], cB_b], writes=bk(0))
                        sc.op(ACT, (lambda ps=ps: se.activation(out=rs2[ps][:, 0:256], in_=banks[0][:, 0:256], func=AF.Ln, bias=EPS, scale=1.0 / 128.0)),
                              reads=bk(0), writes=[rs2_b[ps]])
                        sc.op(ACT, (lambda ps=ps: se.activation(out=rs2[ps][:, 0:256], in_=rs2[ps][:, 0:256], func=AF.Exp, scale=-0.5)),
                              reads=[rs2_b[ps]], writes=[rs2_b[ps]])
                        sc.op(DVE, (lambda ppq=ppq, ps=ps: ve.scalar_tensor_tensor(out=qnf[ps], in0=banks[ppq][:, 0:256], scalar=vecs[:, V_QG:V_QG + 1],
                                                                                   in1=rs2[ps][:, 0:256], op0=ALU.mult, op1=ALU.mult)),
                              reads=bk(ppq) + [rs2_b[ps], vecs_b], writes=[qnf_b[ps]])
                        sc.op(POOL, (lambda ps=ps, ph=ph: ge.tensor_copy(out=qn[:, ph, :], in_=qnf[ps])), reads=[qnf_b[ps]], writes=[qn_b[ph]])
                        if b >= 4:
                            self.gate_pend = (ph, ps)
                    self.pend = cur
                    self.h += 1

                def done(self):
                    return self.h > 9

            def gating(b):
                if b < 4:
                    return
                sc.op(DVE, lambda: ve.tensor_copy(out=gsb.rearrange("p a b -> p (a b)"), in_=banks[0][:, 384:512]), reads=bk(0), writes=[gsb_b])
                sc.op(DVE, (lambda b=b: ve.tensor_tensor(out=cmpb[:, :, :, 0:b],
                                                        in0=gsb[:, :, 0:b].unsqueeze(2).to_broadcast([128, 16, 8, b]),
                                                        in1=gsb[:, :, :].unsqueeze(3).to_broadcast([128, 16, 8, b]), op=ALU.is_gt)),
                      reads=[gsb_b], writes=[cmpb_b])
                sc.op(DVE, (lambda b=b: ve.tensor_reduce(out=rank, in_=cmpb[:, :, :, 0:b], axis=AX.X, op=ALU.add)),
                      reads=[cmpb_b], writes=[rank_b])
                sc.op(DVE, lambda: ve.tensor_scalar(out=nsel, in0=rank, scalar1=3.0, scalar2=NEG, op0=ALU.is_ge, op1=ALU.mult),
                      reads=[rank_b], writes=[nsel_b])
                for j in range(4):
                    bt = rA.next()
                    for q in range(4):
                        m = 4 * j + q
                        sc.op(PE, (lambda bt=bt, q=q, m=m: te.matmul(banks[bt][0:8, q * 128:(q + 1) * 128], lhsT=nsel[:, m, :], rhs=ident_f,
                                                                     start=True, stop=True)),
                              reads=[nsel_b, cF_b], writes=bk(bt))
                    sc.op(ACT, (lambda bt=bt, j=j: se.copy(out=nselT[0:8, 2 * j:2 * j + 2, :].rearrange("p a b -> p (a b)"), in_=banks[bt][0:8, :])),
                          reads=bk(bt), writes=[nselT_b[2 * j], nselT_b[2 * j + 1]])

            q_norm(0)
            qp = QPipe(0)
            while not qp.done():
                qp.step()
            for b in range(8):
                T0 = b * 2
                nxt = None
                if b + 1 < 8:
                    q_norm(b + 1)
                    nxt = QPipe(b + 1)
                nkt = 2 * b + 2
                items = [(h, kt) for h in range(8) for kt in range(nkt)]
                info = {}
                for idx in range(len(items) + LA):
                    if idx < len(items):
                        h, kt = items[idx]
                        kb = kt // 2
                        sbk = rS.next()
                        Sap = banks[sbk][:, 0:256]
                        Sbuf = bk(sbk)
                        own = kb == b
                        a = kt - 2 * b
                        q0 = 128 if (own and a == 1) else 0
                        need_sel = (not own) and b >= 4
                        sc.op(PE, (lambda Sap=Sap, kt=kt, h=h, q0=q0, own=own, need_sel=need_sel: te.matmul(
                            Sap[:, q0:256], lhsT=kT[:, h, kt * 128:(kt + 1) * 128], rhs=qn[:, h, q0:256],
                            start=True, stop=not (own or need_sel))), reads=[kT_b[h][kt // 4], qn_b[h]], writes=Sbuf)
                        if need_sel:
                            sc.op(PE, (lambda Sap=Sap, kb=kb, h=h: te.matmul(Sap, lhsT=ind_full(kb), rhs=nselT[:, h, :], start=False, stop=True)),
                                  reads=[cB_b, nselT_b[h]], writes=Sbuf)
                        if own:
                            sc.op(PE, (lambda Sap=Sap, q0=q0: te.matmul(Sap[:, q0:q0 + 128], lhsT=ident_b, rhs=cmask_b, start=False, stop=True)),
                                  reads=[cB_b], writes=Sbuf)
                        pi = rP.next()
                        sc.op(ACT, (lambda Sap=Sap, pi=pi, q0=q0: se.activation(out=Pb[pi][:, q0:256], in_=Sap[:, q0:256], func=AF.Exp, scale=scale)),
                              reads=Sbuf, writes=[Pb_b[pi]])
                        info[idx] = (pi, q0)
                    if idx >= LA:
                        h, kt = items[idx - LA]
                        pi, q0 = info.pop(idx - LA)
                        ob = 6 + h % 2
                        lastk = kt == nkt - 1
                        sc.op(PE, (lambda ob=ob, pi=pi, kt=kt, h=h, q0=q0: te.matmul(
                            banks[ob][:, q0:256], lhsT=Vv[:, kt, h * 128:(h + 1) * 128], rhs=Pb[pi][:, q0:256],
                            start=(kt == 0), stop=False)), reads=[Vv_b[kt], Pb_b[pi]], writes=bk(ob))
                        sc.op(PE, (lambda ob=ob, pi=pi, kt=kt, q0=q0, lastk=lastk: te.matmul(
                            banks[ob][:, 256 + q0:512], lhsT=ones_b, rhs=Pb[pi][:, q0:256],
                            start=False, stop=lastk)), reads=[cB_b, Pb_b[pi]], writes=bk(ob))
                        if lastk:
                            r = h % 2
                            sc.op(DVE, (lambda ob=ob, r=r: ve.reciprocal(out=rden[r], in_=banks[ob][:, 256:512])), reads=bk(ob), writes=[rden_b[r]])
                            sc.op(DVE, (lambda ob=ob, r=r, h=h: ve.tensor_tensor(out=onT[:, h, :], in0=banks[ob][:, 0:256], in1=rden[r], op=ALU.mult)),
                                  reads=bk(ob) + [rden_b[r]], writes=[onT_b[h]])
                            if nxt is not None:
                                nxt.step()
                if nxt is not None:
                    while not nxt.done():
                        nxt.step()
                    gating(b + 1)
                for dc in range(8):
                    bo = rA.next()
                    for h in range(8):
                        sc.op(PE, (lambda h=h, dc=dc, bo=bo: te.matmul(banks[bo][:, 0:256], lhsT=Wo[:, h, dc * 128:(dc + 1) * 128], rhs=onT[:, h, :],
                                                                       start=(h == 0), stop=(h == 7))),
                              reads=[Wo_b, onT_b[h]], writes=bk(bo))
                    add_to_x(bo, 256, dc, T0)

        def moe_phase():
            sc.barrier()
            A.reset()
            F = FFN(True)
            sq = [A.b(512) for _ in range(2)]
            sq_b = [Buf("sq0"), Buf("sq1")]
            rs = A.f(512)
            rs_b = Buf("rs")
            hf = [A.f(512) for _ in range(2)]
            hf_b = [Buf("hf0"), Buf("hf1")]
            Wr = A.f(8, 8)
            Wr_b = Buf("Wr")
            gates = A.f(16, 8)
            gates_b = [Buf(f"gates{t}") for t in range(16)]
            lgall = A.f(16, 8); dd = A.f(16, 8); eq = A.f(16, 8); lg2 = A.f(16, 8); selm = A.f(16, 8); ex = A.f(16, 8)
            m1 = A.f(16); m2 = A.f(16); den = A.f(16)
            lgall_b, dd_b, eq_b, lg2_b, selm_b, ex_b, m1_b, m2_b, den_b = [Buf(n) for n in ("lgall", "dd", "eq", "lg2", "selm", "ex", "m1", "m2", "den")]
            gall_b = Buf("gates")
            gbc = [A.f(S) for _ in range(2)]
            gbc_b = [[Buf(f"gbc{i}_{g}") for g in range(4)] for i in range(2)]
            sc.op(SP, lambda: sy.dma_start(out=Wr, in_=moe_w_router.rearrange("(k p) n -> p k n", p=128)), writes=[Wr_b], dma_key="Wr")
            for g in range(4):
                T0 = g * 4
                c0 = g * 512
                rms_rstd(lambda c, c0=c0: xT[:, c, c0:c0 + 512], lambda c: xbufs([c], T0, T0 + 4), 8, 512, sq, sq_b, 6, rs, rs_b, 1.0 / D)
                for c in range(8):
                    s = c % 2
                    sc.op(DVE, (lambda c=c, s=s, c0=c0: ve.scalar_tensor_tensor(out=hf[s], in0=xT[:, c, c0:c0 + 512],
                                                                         scalar=vecs[:, V_NF + 8 + c:V_NF + 8 + c + 1], in1=rs,
                                                                         op0=ALU.mult, op1=ALU.mult)),
                          reads=xbufs([c], T0, T0 + 4) + [rs_b, vecs_b], writes=[hf_b[s]])
                    sc.op(ACT, (lambda c=c, s=s, c0=c0: se.copy(out=F.hT[:, c, c0:c0 + 512], in_=hf[s])), reads=[hf_b[s]], writes=[F.hT_b[c][g]])
                    for tt in range(4):
                        sc.op(PE, (lambda c=c, s=s, tt=tt: te.matmul(banks[7][:, tt * 8:(tt + 1) * 8], lhsT=hf[s][:, tt * 128:(tt + 1) * 128],
                                                                     rhs=Wr[:, c, :], start=(c == 0 and tt == 0), stop=(c == 7 and tt == 3))),
                              reads=[hf_b[s], Wr_b], writes=[bk(7)[0]])
                sc.op(DVE, (lambda g=g: ve.tensor_copy(out=lgall[:, 4 * g:4 * g + 4, :].rearrange("p a b -> p (a b)"), in_=banks[7][:, 0:32])),
                      reads=[bk(7)[0]], writes=[lgall_b])
            bc3 = lambda v: v.unsqueeze(2).to_broadcast([128, 16, 8])
            sc.op(DVE, lambda: ve.tensor_reduce(out=m1, in_=lgall, axis=AX.X, op=ALU.max), reads=[lgall_b], writes=[m1_b])
            sc.op(DVE, lambda: ve.tensor_tensor(out=dd, in0=lgall, in1=bc3(m1), op=ALU.subtract), reads=[lgall_b, m1_b], writes=[dd_b])
            sc.op(DVE, lambda: ve.tensor_scalar(out=eq, in0=dd, scalar1=0.0, scalar2=None, op0=ALU.is_ge), reads=[dd_b], writes=[eq_b])
            sc.op(DVE, lambda: ve.scalar_tensor_tensor(out=lg2, in0=eq, scalar=-1e30, in1=dd, op0=ALU.mult, op1=ALU.add),
                  reads=[eq_b, dd_b], writes=[lg2_b])
            sc.op(DVE, lambda: ve.tensor_reduce(out=m2, in_=lg2, axis=AX.X, op=ALU.max), reads=[lg2_b], writes=[m2_b])
            sc.op(DVE, lambda: ve.tensor_tensor(out=selm, in0=dd, in1=bc3(m2), op=ALU.is_ge), reads=[dd_b, m2_b], writes=[selm_b])
            sc.op(ACT, lambda: se.activation(out=ex, in_=dd, func=AF.Exp), reads=[dd_b], writes=[ex_b])
            sc.op(DVE, lambda: ve.tensor_tensor(out=ex, in0=ex, in1=selm, op=ALU.mult), reads=[ex_b, selm_b], writes=[ex_b])
            sc.op(DVE, lambda: ve.tensor_reduce(out=den, in_=ex, axis=AX.X, op=ALU.add), reads=[ex_b], writes=[den_b])
            sc.op(DVE, lambda: ve.reciprocal(out=den, in_=den), reads=[den_b], writes=[den_b])
            sc.op(DVE, lambda: ve.tensor_tensor(out=gates, in0=ex, in1=bc3(den), op=ALU.mult), reads=[ex_b, den_b], writes=[gall_b])

            def build_gbc(e):
                gi = e % 2
                for g in range(4):
                    pb = 6 + g % 2
                    for tt in range(4):
                        t = g * 4 + tt
                        sc.op(PE, (lambda pb=pb, tt=tt, t=t, e=e: te.matmul(banks[pb][:, tt * 128:(tt + 1) * 128],
                                                                            lhsT=gates[:, t, e:e + 1].to_broadcast([128, 128]), rhs=ident_f,
                                                                            start=True, stop=True)),
                              reads=[gall_b, cF_b], writes=bk(pb))
                    sc.op(ACT, (lambda pb=pb, gi=gi, g=g: se.copy(out=gbc[gi][:, g * 512:(g + 1) * 512], in_=banks[pb][:, :])),
                          reads=bk(pb), writes=[gbc_b[gi][g]])

            build_gbc(0)
            for e in range(NE):
                gi = e % 2
                hook = (lambda e=e: build_gbc(e + 1)) if e + 1 < NE else None
                F.run(moe_w_gate[e], moe_w_up[e], moe_w_down[e], gbc=gbc[gi], gbc_b=gbc_b[gi], mid_hook=hook)

        if stage >= 1:
            gla_phase()
        if stage >= 2:
            ffn0_phase()
        if stage >= 3:
            moba_phase()
        if stage >= 4:
            moe_phase()

        sc.barrier()
        A.reset()
        xo = [A.f(1024) for i in range(2)]
        xo_b = [Buf("xo0"), Buf("xo1")]
        for t in range(16):
            s = t % 2
            for half in range(2):
                b = 2 * (t % 2) + half
                for q in range(4):
                    c = half * 4 + q
                    sc.op(PE, (lambda b=b, q=q, c=c, t=t: te.transpose(banks[b][:, q * 128:(q + 1) * 128],
                                                                      xT[:, c, t * 128:(t + 1) * 128], ident_f)),
                          reads=[xT_b[c][t], cF_b], writes=bk(b))
                if half == 0:
                    sc.op(ACT, (lambda b=b, s=s: se.copy(out=xo[s][:, 0:512], in_=banks[b][:, :])), reads=bk(b), writes=[xo_b[s]])
                else:
                    sc.op(DVE, (lambda b=b, s=s: ve.tensor_copy(out=xo[s][:, 512:1024], in_=banks[b][:, :])), reads=bk(b), writes=[xo_b[s]])
            sc.op(SP, (lambda s=s, t=t: sy.dma_start(out=out_d[t * 128:(t + 1) * 128, :], in_=xo[s])),
                  reads=[xo_b[s]], dma_key=f"xo{s}")
        sc.op(SP, None, writes=xo_b)

        block = es.enter_context(nc.Block())
        sc.emit(nc, es, block)
    return nc


_CACHE = {}


def _layout_vecs(inputs):
    v = np.zeros((128, NV), np.float32)
    nm = np.asarray(inputs["norm_mix"], np.float32)
    nf = np.asarray(inputs["norm_ffn"], np.float32)
    for i in range(2):
        v[:, V_NM + i * 8:V_NM + (i + 1) * 8] = nm[i].reshape(8, 128).T
        v[:, V_NF + i * 8:V_NF + (i + 1) * 8] = nf[i].reshape(8, 128).T
    v[:, V_OG:V_OG + 2] = np.asarray(inputs["gla_out_gain"], np.float32)[0].reshape(2, 128).T
    v[:, V_QG] = np.asarray(inputs["moba_q_gain"], np.float32)[0]
    v[:, V_KG] = np.asarray(inputs["moba_k_gain"], np.float32)[0]
    return v


def kernel(_stage=4, _ncores=8, **inputs):
    if _stage not in _CACHE:
        _CACHE[_stage] = build(_stage)
    nc = _CACHE[_stage]
    f = lambda k: np.ascontiguousarray(np.asarray(inputs[k], np.float32))
    x = f("x")
    cf, cb = host_consts()
    shared = {
        "constsf": cf, "constsb": cb,
        "vecs": _layout_vecs(inputs),
        "gla_w_in": f("gla_w_in")[0], "gla_w_gate2": f("gla_w_gate2")[0], "gla_b_gate": f("gla_b_gate"),
        "gla_w_out": f("gla_w_out")[0], "moba_w_qkv": f("moba_w_qkv")[0], "moba_w_out": f("moba_w_out")[0],
        "ffn_w_gate": f("ffn_w_gate")[0], "ffn_w_up": f("ffn_w_up")[0], "ffn_w_down": f("ffn_w_down")[0],
        "moe_w_router": f("moe_w_router")[0], "moe_w_gate": f("moe_w_gate")[0], "moe_w_up": f("moe_w_up")[0],
        "moe_w_down": f("moe_w_down")[0],
    }
    in_maps = []
    for b in range(_ncores):
        m = dict(shared)
        m["x"] = x[b]
        in_maps.append(m)
    res = run_bass_kernel_spmd(nc, in_maps, core_ids=list(range(_ncores)))
    return np.stack([np.asarray(r["out"], np.float32) for r in res.results], axis=0)
```

```python
import numpy as np
from contextlib import ExitStack
import concourse.bass as bass
import concourse.mybir as mybir
from concourse.bass_utils import run_bass_kernel_spmd

F32 = mybir.dt.float32
BF16 = mybir.dt.bfloat16
AF = mybir.ActivationFunctionType
ALU = mybir.AluOpType
AX = mybir.AxisListType

S = 2048
D = 1024
NCH = 8
DFF = 3584
NE = 8
EPS = 1e-6
NEG = -30000.0
SEM_CH = 20000


class Buf:
    __slots__ = ("name", "last_w", "readers")

    def __init__(self, name):
        self.name = name
        self.last_w = None
        self.readers = []


class Op:
    __slots__ = ("eng", "fn", "deps", "signal", "sig", "dma_key", "dval")

    def __init__(self, eng, fn, dma_key):
        self.eng = eng
        self.fn = fn
        self.deps = []
        self.signal = False
        self.sig = 0
        self.dma_key = dma_key
        self.dval = 0


class Sched:
    ENGS = ("pe", "act", "dve", "pool", "sp")

    def __init__(self):
        self.ops = {e: [] for e in self.ENGS}
        self.dma_cnt = {}
        self.last_dma = {}

    def op(self, eng, fn, reads=(), writes=(), dma_key=None):
        o = Op(eng, fn, dma_key)
        deps = {}

        def add(p, kind):
            if p is None:
                return
            if p.dma_key is None and o.dma_key is None and p.eng == eng:
                if eng == "pe":
                    return
            deps[id(p)] = p

        for b in reads:
            add(b.last_w, "raw")
        for b in writes:
            add(b.last_w, "waw")
            for r in b.readers:
                add(r, "war")
        o.deps = list(deps.values())
        for p in o.deps:
            p.signal = True
        for b in reads:
            if o.dma_key is None:
                b.readers = [r for r in b.readers if not (r.dma_key is None and r.eng == eng)]
            b.readers.append(o)
        for b in writes:
            b.last_w = o
            b.readers = []
        if dma_key is not None:
            self.dma_cnt[dma_key] = self.dma_cnt.get(dma_key, 0) + 16
            o.dval = self.dma_cnt[dma_key]
            self.last_dma[dma_key] = o
        self.ops[eng].append(o)
        return o

    def barrier(self):
        lasts = []
        for e in self.ENGS:
            for p in reversed(self.ops[e]):
                if p.dma_key is None and p.fn is not None:
                    lasts.append(p)
                    break
        lasts += list(self.last_dma.values())
        for e in self.ENGS:
            o = Op(e, None, None)
            o.deps = [p for p in lasts if not (p.dma_key is None and p.eng == e)]
            for p in o.deps:
                p.signal = True
            self.ops[e].append(o)

    def finalize(self):
        self.nsig = {}
        for e in self.ENGS:
            n = 0
            for o in self.ops[e]:
                if o.dma_key is None and o.signal:
                    n += 1
                    o.sig = n
            self.nsig[e] = n

    def emit(self, nc, es, block):
        self.finalize()
        engsem = {}
        for e in self.ENGS:
            nchunks = (self.nsig[e] + SEM_CH - 1) // SEM_CH
            engsem[e] = [es.enter_context(nc.semaphore(f"s_{e}_{i}")) for i in range(max(nchunks, 1))]
        dmasem = {k: es.enter_context(nc.semaphore(f"d_{k}")) for k in self.dma_cnt}
        handles = {"pe": nc.tensor, "act": nc.scalar, "dve": nc.vector, "pool": nc.gpsimd, "sp": nc.sync}

        def semval(p):
            if p.dma_key is not None:
                return dmasem[p.dma_key], p.dval
            return engsem[p.eng][(p.sig - 1) // SEM_CH], (p.sig - 1) % SEM_CH + 1

        def run(e):
            h = handles[e]
            waited = {}
            for o in self.ops[e]:
                need = {}
                for p in o.deps:
                    s, v = semval(p)
                    k = id(s)
                    if v > waited.get(k, 0) and v > need.get(k, (None, 0))[1]:
                        need[k] = (s, v)
                if o.dma_key is not None and o.dval > 16:
                    s = dmasem[o.dma_key]
                    if o.dval - 16 > waited.get(id(s), 0) and o.dval - 16 > need.get(id(s), (None, 0))[1]:
                        need[id(s)] = (s, o.dval - 16)
                for k, (s, v) in need.items():
                    h.wait_ge(s, v)
                    waited[k] = v
                if o.fn is None:
                    continue
                ins = o.fn()
                if o.dma_key is not None:
                    ins.then_inc(dmasem[o.dma_key], 16)
                elif o.signal:
                    s, _ = semval(o)
                    ins.then_inc(s, 1)

        block.tensor(lambda eng: run("pe"))
        block.scalar(lambda eng: run("act"))
        block.vector(lambda eng: run("dve"))
        block.gpsimd(lambda eng: run("pool"))
        block.sync(lambda eng: run("sp"))


class Rot:
    def __init__(self, items):
        self.items = items
        self.i = 0

    def next(self):
        x = self.items[self.i % len(self.items)]
        self.i += 1
        return x


def host_consts():
    j = np.arange(128)[:, None]
    i = np.arange(128)[None, :]
    same = (j // 64) == (i // 64)
    ident = (j == i).astype(np.float32)
    tri = (same & (j <= i)).astype(np.float32)
    triu = (same & (j > i)).astype(np.float32)
    ones = np.ones((128, 128), np.float32)
    cmask = np.where(j <= i, 0.0, NEG).astype(np.float32)
    ind = np.zeros((128, 8, 128), np.float32)
    for b in range(8):
        ind[b, b, :] = 1.0
    cf = np.concatenate([ident, tri, triu], axis=1)
    cb = np.concatenate([ident, ones, cmask, ind.reshape(128, 1024)], axis=1)
    return np.ascontiguousarray(cf), np.ascontiguousarray(cb)


NCF, NCB = 384, 1408
V_NM, V_NF, V_OG, V_QG, V_KG = 0, 16, 32, 34, 35
NV = 36
NA = 70400


def build(stage=4):
    nc = bass.Bass("TRN2", target_bir_lowering=False)
    dt = nc.dram_tensor
    x_d = dt("x", [S, D], F32, kind="ExternalInput").ap()
    cf_d = dt("constsf", [128, NCF], F32, kind="ExternalInput").ap()
    cb_d = dt("constsb", [128, NCB], F32, kind="ExternalInput").ap()
    vecs_d = dt("vecs", [128, NV], F32, kind="ExternalInput").ap()
    gla_w_in = dt("gla_w_in", [D, 3088], F32, kind="ExternalInput").ap()
    gla_w_gate2 = dt("gla_w_gate2", [16, 512], F32, kind="ExternalInput").ap()
    gla_b_gate = dt("gla_b_gate", [1, 512], F32, kind="ExternalInput").ap()
    gla_w_out = dt("gla_w_out", [D, D], F32, kind="ExternalInput").ap()
    moba_w_qkv = dt("moba_w_qkv", [D, 3072], F32, kind="ExternalInput").ap()
    moba_w_out = dt("moba_w_out", [D, D], F32, kind="ExternalInput").ap()
    ffn_w_gate = dt("ffn_w_gate", [D, DFF], F32, kind="ExternalInput").ap()
    ffn_w_up = dt("ffn_w_up", [D, DFF], F32, kind="ExternalInput").ap()
    ffn_w_down = dt("ffn_w_down", [DFF, D], F32, kind="ExternalInput").ap()
    moe_w_router = dt("moe_w_router", [D, NE], F32, kind="ExternalInput").ap()
    moe_w_gate = dt("moe_w_gate", [NE, D, DFF], F32, kind="ExternalInput").ap()
    moe_w_up = dt("moe_w_up", [NE, D, DFF], F32, kind="ExternalInput").ap()
    moe_w_down = dt("moe_w_down", [NE, DFF, D], F32, kind="ExternalInput").ap()
    out_d = dt("out", [S, D], F32, kind="ExternalOutput").ap()

    sc = Sched()
    PE, ACT, DVE, POOL, SP = "pe", "act", "dve", "pool", "sp"
    te, se, ve, ge, sy = nc.tensor, nc.scalar, nc.vector, nc.gpsimd, nc.sync

    with ExitStack() as es:
        def sb(name, shape, dtype):
            return es.enter_context(nc.sbuf_tensor("sb_" + name, shape, dtype))

        xT = sb("xT", [128, NCH, S], F32)
        xT_b = [[Buf(f"xT{c}_{t}") for t in range(16)] for c in range(NCH)]
        cF = sb("cF", [128, NCF], F32)
        cB = sb("cB", [128, NCB], BF16)
        cF_b, cB_b = Buf("cF"), Buf("cB")
        vecs = sb("vecs", [128, NV], F32)
        vecs_b = Buf("vecs")
        arena = sb("arena", [128, NA], BF16)
        banks = [es.enter_context(nc.psum_tensor(f"bank{i}", [128, 512], F32)) for i in range(8)]
        bank_b = [[b_, b_] for b_ in [Buf(f"bank{i}") for i in range(8)]]

        class Arena:
            def __init__(self):
                self.off = 0

            def reset(self):
                self.off = 0

            def _shape(self, v, shape):
                if len(shape) == 2:
                    return v.rearrange("p (a b) -> p a b", a=shape[0])
                if len(shape) == 3:
                    return v.rearrange("p (a b c) -> p a b c", a=shape[0], b=shape[1])
                return v

            def b(self, *shape, parts=128):
                n = int(np.prod(shape))
                assert self.off + n <= NA, (self.off, n)
                v = arena[0:parts, self.off:self.off + n]
                self.off += n
                return self._shape(v, shape)

            def f(self, *shape, parts=128):
                n = int(np.prod(shape))
                self.off += self.off % 2
                assert self.off + 2 * n <= NA, (self.off, n)
                v = arena[0:parts, self.off:self.off + 2 * n].bitcast(F32)
                self.off += 2 * n
                return self._shape(v, shape)

        A = Arena()
        ident_f = cF[:, 0:128]
        tri_f = cF[:, 128:256]
        triu_f = cF[:, 256:384]
        ident_b = cB[:, 0:128]
        ones_b = cB[:, 128:256]
        cmask_b = cB[:, 256:384]

        def ind_b(kb):
            return cB[0:8, 384 + kb * 128:384 + (kb + 1) * 128]

        def ind_full(kb):
            return cB[:, 384 + kb * 128:384 + (kb + 1) * 128]

        def xbufs(cs, t0, t1):
            return [xT_b[c][t] for c in cs for t in range(t0, t1)]

        def bk(i):
            return bank_b[i]

        sc.op(SP, lambda: sy.dma_start(out=cF[:], in_=cf_d[:, :]), writes=[cF_b], dma_key="cF")
        sc.op(POOL, lambda: ge.dma_start(out=cB[:], in_=cb_d[:, :]), writes=[cB_b], dma_key="cB")
        sc.op(SP, lambda: sy.dma_start(out=vecs[:], in_=vecs_d[:, :]), writes=[vecs_b], dma_key="vecs")

        A.off = NA - 8192
        xin = [A.f(1024) for i in range(4)]
        xin_b = [Buf(f"xin{i}") for i in range(4)]
        for t in range(16):
            s = t % 4
            sc.op(SP, (lambda s=s, t=t: sy.dma_start(out=xin[s], in_=x_d[t * 128:(t + 1) * 128, :])),
                  writes=[xin_b[s]], dma_key=f"xin{s}")
            for half in range(2):
                b = 2 * (t % 4) + half
                for q in range(4):
                    c = half * 4 + q
                    sc.op(PE, (lambda b=b, q=q, c=c, s=s: te.transpose(banks[b][:, q * 128:(q + 1) * 128],
                                                                      xin[s][:, c * 128:(c + 1) * 128], ident_f)),
                          reads=[xin_b[s], cF_b], writes=bk(b))
                if half == 0:
                    fn = (lambda b=b, t=t: se.copy(out=xT[:, 0:4, t * 128:(t + 1) * 128],
                                                   in_=banks[b][:].rearrange("p (a b) -> p a b", a=4)))
                    sc.op(ACT, fn, reads=bk(b), writes=xbufs(range(0, 4), t, t + 1))
                else:
                    fn = (lambda b=b, t=t: ve.tensor_copy(out=xT[:, 4:8, t * 128:(t + 1) * 128],
                                                          in_=banks[b][:].rearrange("p (a b) -> p a b", a=4)))
                    sc.op(DVE, fn, reads=bk(b), writes=xbufs(range(4, 8), t, t + 1))

        def load_w(dst, dst_b, src, key):
            sc.op(POOL, lambda: ge.dma_start(out=dst, in_=src), writes=[dst_b], dma_key=key)

        def rms_rstd(src_fn, src_bufs, nchunks, n, sq, sq_b, ssbank, rs_t, rs_b, inv_n):
            for c in range(nchunks):
                s = c % 2
                sc.op(ACT, (lambda c=c, s=s: se.activation(out=sq[s], in_=src_fn(c), func=AF.Square)),
                      reads=src_bufs(c), writes=[sq_b[s]])
                sc.op(PE, (lambda c=c, s=s: te.matmul(banks[ssbank][:, 0:n], lhsT=ones_b, rhs=sq[s],
                                                      start=(c == 0), stop=(c == nchunks - 1))),
                      reads=[sq_b[s], cB_b], writes=bk(ssbank))
            sc.op(ACT, lambda: se.activation(out=rs_t, in_=banks[ssbank][:, 0:n], func=AF.Ln, bias=EPS, scale=inv_n),
                  reads=bk(ssbank), writes=[rs_b])
            sc.op(ACT, lambda: se.activation(out=rs_t, in_=rs_t, func=AF.Exp, scale=-0.5), reads=[rs_b], writes=[rs_b])

        def norm_to(n, t0, gcol, h_fn, h_bufs, sq, sq_b, ssbank, rs_t, rs_b):
            nt = n // 128
            c0 = t0 * 128
            rms_rstd(lambda c: xT[:, c, c0:c0 + n], lambda c: xbufs([c], t0, t0 + nt), 8, n, sq, sq_b, ssbank, rs_t, rs_b, 1.0 / D)
            for c in range(8):
                sc.op(DVE, (lambda c=c: ve.scalar_tensor_tensor(out=h_fn(c), in0=xT[:, c, c0:c0 + n],
                                                                scalar=vecs[:, gcol + c:gcol + c + 1], in1=rs_t,
                                                                op0=ALU.mult, op1=ALU.mult)),
                      reads=xbufs([c], t0, t0 + nt) + [rs_b, vecs_b], writes=h_bufs(c))

        def add_to_x(bank, n, dc, t0):
            nt = n // 128
            c0 = t0 * 128
            sc.op(DVE, lambda: ve.tensor_tensor(out=xT[:, dc, c0:c0 + n], in0=xT[:, dc, c0:c0 + n],
                                                in1=banks[bank][:, 0:n], op=ALU.add),
                  reads=bk(bank) + xbufs([dc], t0, t0 + nt), writes=xbufs([dc], t0, t0 + nt))

        def gla_phase():
            A.reset()
            GN = 256
            Win = A.b(8, 3088)
            Wout = A.b(8, 1024)
            hTg = A.b(8, GN)
            sq = [A.b(GN) for _ in range(2)]
            qdec = A.b(4, GN)
            kinv = A.b(4, GN)
            kend = [A.b(512) for _ in range(2)]
            Vt = [A.b(1024) for _ in range(2)]
            onT = A.b(8, GN)
            STm = [A.b(128) for _ in range(2)]
            stbf = [A.b(256) for _ in range(4)]
            osq = [A.b(2, 128) for _ in range(4)]
            sg2 = [A.b(8, GN), A.b(8, GN)]
            E1 = A.f(4, GN)
            E2 = A.f(4, GN)
            E3 = [A.f(512) for _ in range(2)]
            Lt = [A.f(512) for _ in range(2)]
            tmpE = A.f(512)
            stf = [A.f(256) for _ in range(4)]
            rsg = A.f(GN)
            ors4 = A.f(512)
            ot1 = [A.f(128) for _ in range(2)]
            a_aug = A.f(GN, parts=17)
            W2aug = A.f(512, parts=17)
            Win_b = [Buf(f"Win{k}") for k in range(8)]
            Wout_b, hTg_b, qdec_b, kinv_b = Buf("Wout"), Buf("hTg"), Buf("qdec"), Buf("kinv")
            sq_b = [Buf("sq0"), Buf("sq1")]
            kend_b = [Buf("kend0"), Buf("kend1")]
            Vt_b = [Buf("Vt0"), Buf("Vt1")]
            onT_b = [Buf(f"onT{t}") for t in range(2)]
            STm_b = [Buf("STm0"), Buf("STm1")]
            stbf_b = [Buf(f"stbf{i}") for i in range(4)]
            stf_b = [Buf(f"stf{i}") for i in range(4)]
            osq_b = [Buf(f"osq{i}") for i in range(4)]
            ors4_b = Buf("ors4")
            E1_b = [Buf(f"E1_{t}") for t in range(2)]
            E2_b = [Buf(f"E2_{t}") for t in range(2)]
            rsg_b = Buf("rsg")
            sg2_b = [Buf("sg0"), Buf("sg1")]
            ors_b = [Buf("ors0"), Buf("ors1")]
            ot1_b = [Buf("ot10"), Buf("ot11")]
            Lt_b = [Buf("L0"), Buf("L1")]
            tmpE_b = Buf("tmpE")
            E3_b = [Buf("E30"), Buf("E31")]
            aaug_b, W2_b = Buf("aaug"), Buf("W2aug")

            for k in range(8):
                load_w(Win[:, k, :], Win_b[k], gla_w_in[k * 128:(k + 1) * 128, :], f"Win{k}")
            load_w(Wout, Wout_b, gla_w_out.rearrange("(k p) n -> p k n", p=128), "Wout")
            sc.barrier()
            sc.op(SP, lambda: sy.dma_start(out=W2aug[0:16, :], in_=gla_w_gate2[:, :]), writes=[W2_b], dma_key="W2")
            sc.op(SP, lambda: sy.dma_start(out=W2aug[16:17, :], in_=gla_b_gate[:, :]), writes=[W2_b], dma_key="W2")
            sc.op(POOL, lambda: ge.memset(a_aug, 1.0), writes=[aaug_b])
            for h in range(4):
                sc.op(POOL, (lambda h=h: ge.memset(stf[h], 0.0)), writes=[stf_b[h]])
                sc.op(POOL, (lambda h=h: ge.memset(stbf[h], 0.0)), writes=[stbf_b[h]])

            rA = Rot([0, 1])
            rB = Rot([2, 3])
            rC = Rot([4, 5])
            rF = Rot([2, 3, 4, 5])
            def prenorm1a(g):
                T0 = g * 2
                c0 = T0 * 128
                rms_rstd(lambda c: xT[:, c, c0:c0 + GN], lambda c: xbufs([c], T0, T0 + 2), 8, GN, sq, sq_b, rA.next(), rsg, rsg_b, 1.0 / D)

            def prenorm1b(g, hTg, hTg_b):
                T0 = g * 2
                c0 = T0 * 128
                for c in range(8):
                    sc.op(DVE, (lambda c=c: ve.scalar_tensor_tensor(out=hTg[:, c, :], in0=xT[:, c, c0:c0 + GN],
                                                                    scalar=vecs[:, V_NM + c:V_NM + c + 1], in1=rsg,
                                                                    op0=ALU.mult, op1=ALU.mult)),
                          reads=xbufs([c], T0, T0 + 2) + [rsg_b, vecs_b], writes=[hTg_b])

            def prenorm2(g, hTg, hTg_b):
                ba = rA.next()
                for k in range(8):
                    sc.op(PE, (lambda k=k, ba=ba: te.matmul(banks[ba][0:16, 0:GN], lhsT=Win[:, k, 3072:3088], rhs=hTg[:, k, :],
                                                            start=(k == 0), stop=(k == 7))),
                          reads=[Win_b[k], hTg_b], writes=bk(ba))
                sc.op(ACT, (lambda ba=ba: se.copy(out=a_aug[0:16, :], in_=banks[ba][0:16, 0:GN])), reads=bk(ba), writes=[aaug_b])

            def do_group(g, hTg, hTg_b):
                T0 = g * 2
                sg, sg_b = sg2[g % 2], sg2_b[g % 2]

                def g_chunk(c, rot=None):
                    bq = (rot or rF).next()
                    for k in range(8):
                        sc.op(PE, (lambda bq=bq, k=k, c=c: te.matmul(banks[bq][:, 0:GN], lhsT=Win[:, k, 2048 + c * 128:2048 + (c + 1) * 128],
                                                                     rhs=hTg[:, k, :], start=(k == 0), stop=(k == 7))),
                              reads=[Win_b[k], hTg_b], writes=bk(bq))
                    sc.op(ACT, (lambda bq=bq, c=c: se.activation(out=sg[:, c, :], in_=banks[bq][:, 0:GN], func=AF.Silu)),
                          reads=bk(bq), writes=[sg_b])

                def z_part(t):
                    tc0 = t * 128
                    bz = rA.next()
                    sc.op(PE, (lambda bz=bz, tc0=tc0: te.matmul(banks[bz][:, :], lhsT=a_aug[0:17, tc0:tc0 + 128], rhs=W2aug[0:17, :],
                                                                start=True, stop=True)),
                          reads=[aaug_b, W2_b], writes=bk(bz))
                    sc.op(ACT, (lambda bz=bz: se.activation(out=tmpE, in_=banks[bz][:, :], func=AF.Exp, scale=-1.0)),
                          reads=bk(bz), writes=[tmpE_b])
                    sc.op(ACT, (lambda t=t: se.activation(out=Lt[t], in_=tmpE, func=AF.Ln, bias=1.0, scale=1.0)),
                          reads=[tmpE_b], writes=[Lt_b[t]])

                def cl_rl(t):
                    tc0 = t * 128
                    bc = rF.next()
                    for h in range(4):
                        sc.op(PE, (lambda bc=bc, h=h, t=t: te.matmul(banks[bc][:, h * 128:(h + 1) * 128],
                                                                     lhsT=Lt[t][:, h * 128:(h + 1) * 128], rhs=tri_f,
                                                                     start=True, stop=True)),
                              reads=[Lt_b[t], cF_b], writes=[bk(bc)[h // 2]])
                    sc.op(ACT, (lambda bc=bc, tc0=tc0: se.activation(out=E1[:, :, tc0:tc0 + 128],
                                                                     in_=banks[bc][:].rearrange("p (a b) -> p a b", a=4),
                                                                     func=AF.Exp, scale=-1.0 / 16.0)),
                          reads=bk(bc), writes=[E1_b[t]])
                    sc.op(ACT, (lambda bc=bc, tc0=tc0: se.activation(out=E2[:, :, tc0:tc0 + 128],
                                                                     in_=banks[bc][:].rearrange("p (a b) -> p a b", a=4),
                                                                     func=AF.Exp, scale=1.0 / 16.0)),
                          reads=bk(bc), writes=[E2_b[t]])
                    br = rA.next()
                    sc.op(PE, (lambda br=br, t=t: te.matmul(banks[br][:, :], lhsT=triu_f, rhs=Lt[t], start=True, stop=True)),
                          reads=[Lt_b[t], cF_b], writes=bk(br))
                    sc.op(ACT, (lambda br=br, t=t: se.activation(out=E3[t], in_=banks[br][:, :], func=AF.Exp, scale=-1.0 / 16.0)),
                          reads=bk(br), writes=[E3_b[t]])

                def kv_tok_v(t):
                    kv_tok(t, (("v0", 1024), ("v1", 1536)))

                def kv_tok_k(t):
                    kv_tok(t, (("k", 512),))

                def kv_tok(t, kinds):
                    tc0 = t * 128
                    for (kind, col0) in kinds:
                        bb = rF.next()
                        for k in range(8):
                            sc.op(PE, (lambda bb=bb, k=k, col0=col0, tc0=tc0: te.matmul(banks[bb][:, :], lhsT=hTg[:, k, tc0:tc0 + 128],
                                                                                        rhs=Win[:, k, col0:col0 + 512],
                                                                                        start=(k == 0), stop=(k == 7))),
                                  reads=[Win_b[k], hTg_b], writes=bk(bb))
                        if kind == "k":
                            sc.op(DVE, (lambda bb=bb, t=t: ve.tensor_tensor(out=kend[t], in0=banks[bb][:, :], in1=E3[t], op=ALU.mult)),
                                  reads=bk(bb) + [E3_b[t]], writes=[kend_b[t]])
                        elif kind == "v0":
                            sc.op(ACT, (lambda bb=bb, t=t: se.copy(out=Vt[t][:, 0:512], in_=banks[bb][:, :])),
                                  reads=bk(bb), writes=[Vt_b[t]])
                        else:
                            sc.op(ACT, (lambda bb=bb, t=t: se.copy(out=Vt[t][:, 512:1024], in_=banks[bb][:, :])),
                                  reads=bk(bb), writes=[Vt_b[t]])

                def qk_head(h):
                    bq = rF.next()
                    for k in range(8):
                        sc.op(PE, (lambda bq=bq, k=k, h=h: te.matmul(banks[bq][:, 0:GN], lhsT=Win[:, k, h * 128:(h + 1) * 128], rhs=hTg[:, k, :],
                                                                     start=(k == 0), stop=(k == 7))),
                              reads=[Win_b[k], hTg_b], writes=bk(bq))
                    sc.op(DVE, (lambda bq=bq, h=h: ve.scalar_tensor_tensor(out=qdec[:, h, :], in0=banks[bq][:, 0:GN], scalar=128.0 ** -0.5,
                                                                           in1=E1[:, h, :], op0=ALU.mult, op1=ALU.mult)),
                          reads=bk(bq) + E1_b, writes=[qdec_b])
                    bq = rF.next()
                    for k in range(8):
                        sc.op(PE, (lambda bq=bq, k=k, h=h: te.matmul(banks[bq][:, 0:GN], lhsT=Win[:, k, 512 + h * 128:512 + (h + 1) * 128],
                                                                     rhs=hTg[:, k, :], start=(k == 0), stop=(k == 7))),
                              reads=[Win_b[k], hTg_b], writes=bk(bq))
                    sc.op(DVE, (lambda bq=bq, h=h: ve.tensor_tensor(out=kinv[:, h, :], in0=banks[bq][:, 0:GN], in1=E2[:, h, :], op=ALU.mult)),
                          reads=bk(bq) + E2_b, writes=[kinv_b])

                def F1():
                    for c in range(8):
                        g_chunk(c, rot=rA)

                def F2():
                    z_part(0); z_part(1)
                    kv_tok_v(0)
                    cl_rl(0); cl_rl(1)
                    kv_tok_v(1)
                    kv_tok_k(0); kv_tok_k(1)
                    for h in range(4):
                        qk_head(h)

                def R(t):
                    tc0 = t * 128
                    obase = 6 if t == 0 else 2
                    for h in range(4):
                        half = h % 2
                        sm = h % 2
                        b5 = rC.next()
                        sc.op(PE, (lambda h=h, b5=b5, tc0=tc0: te.matmul(banks[b5][:, 0:128],
                                                                         lhsT=kinv[:, h, tc0:tc0 + 128], rhs=qdec[:, h, tc0:tc0 + 128],
                                                                         start=True, stop=True)),
                              reads=[kinv_b, qdec_b], writes=bk(b5))
                        sc.op(DVE, (lambda b5=b5, sm=sm: ve.tensor_tensor(out=STm[sm], in0=banks[b5][:, 0:128],
                                                                          in1=tri_f, op=ALU.mult)),
                              reads=bk(b5) + [cF_b], writes=[STm_b[sm]])
                        for vc in range(2):
                            ob = obase + h // 2
                            oc = ((h % 2) * 2 + vc) * 128
                            sc.op(PE, (lambda ob=ob, oc=oc, t=t, h=h, vc=vc, sm=sm: te.matmul(
                                banks[ob][:, oc:oc + 128], lhsT=Vt[t][:, h * 256 + vc * 128:h * 256 + (vc + 1) * 128], rhs=STm[sm],
                                start=(h % 2 == 0 and vc == 0), stop=False)), reads=[Vt_b[t], STm_b[sm]], writes=[bk(ob)[h % 2]])
                    for cc in range(2):
                        cs = tc0 + cc * 64
                        for h in range(4):
                            for vc in range(2):
                                ob = obase + h // 2
                                oc = ((h % 2) * 2 + vc) * 128 + cc * 64
                                sc.op(PE, (lambda ob=ob, oc=oc, h=h, vc=vc, cs=cs, cc=cc: te.matmul(
                                    banks[ob][:, oc:oc + 64], lhsT=stbf[h][:, vc * 128:(vc + 1) * 128], rhs=qdec[:, h, cs:cs + 64],
                                    start=False, stop=(cc == 1 and h % 2 == 1 and vc == 1))), reads=[stbf_b[h], qdec_b], writes=[bk(ob)[h % 2]])
                        for h in range(4):
                            half = h % 2
                            b5 = rC.next()
                            sc.op(PE, (lambda h=h, b5=b5, t=t, cc=cc: te.matmul(
                                banks[b5][:, 0:256], lhsT=kend[t][cc * 64:(cc + 1) * 64, h * 128:(h + 1) * 128],
                                rhs=Vt[t][cc * 64:(cc + 1) * 64, h * 256:(h + 1) * 256], start=True, stop=True)),
                                reads=[kend_b[t], Vt_b[t]], writes=bk(b5))
                            sc.op(DVE, (lambda h=h, b5=b5, cs=cs: ve.scalar_tensor_tensor(
                                out=stf[h], in0=stf[h], scalar=E1[:, h, cs + 63:cs + 64], in1=banks[b5][:, 0:256],
                                op0=ALU.mult, op1=ALU.add)), reads=[stf_b[h], E1_b[t]] + bk(b5), writes=[stf_b[h]])
                            sc.op(ACT, (lambda h=h: se.copy(out=stbf[h], in_=stf[h])), reads=[stf_b[h]], writes=[stbf_b[h]])
                def ON_s(t):
                    obase = 6 if t == 0 else 2
                    bs = rA.next()
                    for h in range(4):
                        ob = obase + h // 2
                        oc = (h % 2) * 256
                        sc.op(ACT, (lambda ob=ob, oc=oc, h=h: se.activation(out=osq[h],
                                                                            in_=banks[ob][:, oc:oc + 256].rearrange("p (a b) -> p a b", a=2),
                                                                            func=AF.Square)),
                              reads=[bk(ob)[h % 2]], writes=[osq_b[h]])
                    for h in range(4):
                        for vc in range(2):
                            sc.op(PE, (lambda bs=bs, vc=vc, h=h: te.matmul(banks[bs][:, h * 128:(h + 1) * 128], lhsT=ones_b, rhs=osq[h][:, vc, :],
                                                                           start=(h == 0 and vc == 0), stop=(h == 3 and vc == 1))),
                                  reads=[osq_b[h], cB_b], writes=bk(bs))
                    sc.op(ACT, (lambda bs=bs: se.activation(out=ors4, in_=banks[bs][:, :], func=AF.Ln, bias=EPS, scale=1.0 / 256.0)),
                          reads=bk(bs), writes=[ors4_b])
                    sc.op(ACT, lambda: se.activation(out=ors4, in_=ors4, func=AF.Exp, scale=-0.5), reads=[ors4_b], writes=[ors4_b])

                def ON_a(t):
                    obase = 6 if t == 0 else 2
                    tc0 = t * 128
                    for h in range(4):
                        ob = obase + h // 2
                        oc = (h % 2) * 256
                        for vc in range(2):
                            c = h * 2 + vc
                            s1 = vc
                            sc.op(DVE, (lambda ob=ob, oc=oc, vc=vc, s1=s1, h=h: ve.scalar_tensor_tensor(
                                out=ot1[s1], in0=banks[ob][:, oc + vc * 128:oc + (vc + 1) * 128], scalar=vecs[:, V_OG + vc:V_OG + vc + 1],
                                in1=ors4[:, h * 128:(h + 1) * 128], op0=ALU.mult, op1=ALU.mult)),
                                reads=[bk(ob)[h % 2], ors4_b, vecs_b], writes=[ot1_b[s1]])
                            sc.op(DVE, (lambda c=c, s1=s1, tc0=tc0: ve.tensor_tensor(out=onT[:, c, tc0:tc0 + 128], in0=ot1[s1],
                                                                                     in1=sg[:, c, tc0:tc0 + 128], op=ALU.mult)),
                                  reads=[ot1_b[s1], sg_b], writes=[onT_b[t]])
                def O():
                    for dc in range(8):
                        bo = rC.next()
                        for c in range(8):
                            sc.op(PE, (lambda bo=bo, c=c, dc=dc: te.matmul(banks[bo][:, 0:GN], lhsT=Wout[:, c, dc * 128:(dc + 1) * 128], rhs=onT[:, c, :],
                                                                           start=(c == 0), stop=(c == 7))),
                                  reads=[Wout_b] + onT_b, writes=bk(bo))
                        add_to_x(bo, GN, dc, T0)

                return F1, F2, R, ON_s, ON_a, O

            hT2 = [hTg, A.b(8, GN)]
            hT2_b = [hTg_b, Buf("hTg1")]
            NG = S // GN
            stages = [do_group(g, hT2[g % 2], hT2_b[g % 2]) for g in range(NG)]
            prenorm1a(0)
            prenorm1b(0, hT2[0], hT2_b[0])
            prenorm2(0, hT2[0], hT2_b[0])
            stages[0][0]()
            stages[0][1]()
            for g in range(NG):
                F1, F2, R, ON_s, ON_a, O = stages[g]
                nx = g + 1 < NG
                nh, nhb = hT2[(g + 1) % 2], hT2_b[(g + 1) % 2]
                if nx:
                    prenorm1a(g + 1)
                R(0)
                ON_s(0)
                if nx:
                    prenorm1b(g + 1, nh, nhb)
                    prenorm2(g + 1, nh, nhb)
                R(1)
                ON_a(0)
                ON_s(1)
                ON_a(1)
                if nx:
                    stages[g + 1][0]()
                O()
                if nx:
                    stages[g + 1][1]()

        class FFN:
            def __init__(self, with_gate):
                self.hT = A.b(8, S)
                self.hT_b = [[Buf(f"hT{c}_{g}") for g in range(4)] for c in range(8)]
                self.act = A.b(4, S)
                self.act_b = [[Buf(f"act{f}_{g}") for g in range(4)] for f in range(4)]
                self.Wg = [A.b(8, 512) for _ in range(2)]
                self.Wu = [A.b(8, 512) for _ in range(2)]
                self.Wd = [A.b(4, 1024) for _ in range(2)]
                self.Wg_b = [Buf("Wg0"), Buf("Wg1")]
                self.Wu_b = [Buf("Wu0"), Buf("Wu1")]
                self.Wd_b = [Buf("Wd0"), Buf("Wd1")]
                self.sil = [A.f(512) for _ in range(3)]
                self.sil_b = [Buf(f"sil{i}") for i in range(3)]
                self.rsil = Rot([0, 1, 2])
                if with_gate:
                    self.sil2 = [A.f(512) for _ in range(2)]
                    self.sil2_b = [Buf(f"sil2{i}") for i in range(2)]
                    self.rsil2 = Rot([0, 1])
                self.rGU = Rot([0, 1, 2, 3, 4])
                self.rD = Rot([5, 6, 7])
                self.cnt = 0

            def run(self, wg, wu, wd, gbc=None, gbc_b=None, mid_hook=None):
                hT, act = self.hT, self.act
                for scn in range(7):
                    s = self.cnt % 2
                    self.cnt += 1
                    load_w(self.Wg[s], self.Wg_b[s], wg[:, scn * 512:(scn + 1) * 512].rearrange("(k p) n -> p k n", p=128), f"Wg{s}")
                    load_w(self.Wu[s], self.Wu_b[s], wu[:, scn * 512:(scn + 1) * 512].rearrange("(k p) n -> p k n", p=128), f"Wu{s}")
                    load_w(self.Wd[s], self.Wd_b[s], wd[scn * 512:(scn + 1) * 512, :].rearrange("(f p) n -> p f n", p=128), f"Wd{s}")
                    Wg, Wu, Wd = self.Wg[s], self.Wu[s], self.Wd[s]
                    for f in range(4):
                        for tg in range(4):
                            c0 = tg * 512
                            pg, pu = self.rGU.next(), self.rGU.next()
                            for (pb, W, Wb) in ((pg, Wg, self.Wg_b[s]), (pu, Wu, self.Wu_b[s])):
                                for k in range(8):
                                    sc.op(PE, (lambda pb=pb, W=W, k=k, f=f, c0=c0: te.matmul(
                                        banks[pb][:, :], lhsT=W[:, k, f * 128:(f + 1) * 128], rhs=hT[:, k, c0:c0 + 512],
                                        start=(k == 0), stop=(k == 7))), reads=[Wb, self.hT_b[k][tg]], writes=bk(pb))
                            si = self.rsil.next()
                            sc.op(ACT, (lambda pg=pg, si=si: se.activation(out=self.sil[si], in_=banks[pg][:, :], func=AF.Silu)),
                                  reads=bk(pg), writes=[self.sil_b[si]])
                            src, src_b = self.sil[si], self.sil_b[si]
                            if gbc is not None:
                                s2 = self.rsil2.next()
                                sc.op(POOL, (lambda si=si, s2=s2, c0=c0: ge.tensor_tensor(out=self.sil2[s2], in0=self.sil[si],
                                                                                          in1=gbc[:, c0:c0 + 512], op=ALU.mult)),
                                      reads=[self.sil_b[si], gbc_b[tg]], writes=[self.sil2_b[s2]])
                                src, src_b = self.sil2[s2], self.sil2_b[s2]
                            sc.op(DVE, (lambda src=src, pu=pu, f=f, c0=c0: ve.tensor_tensor(out=act[:, f, c0:c0 + 512], in0=src,
                                                                                            in1=banks[pu][:, :], op=ALU.mult)),
                                  reads=[src_b] + bk(pu), writes=[self.act_b[f][tg]])
                    if mid_hook is not None and scn == 3:
                        mid_hook()
                    for tg in range(4):
                        for dc in range(8):
                            c0 = tg * 512
                            pd = self.rD.next()
                            for f in range(4):
                                sc.op(PE, (lambda pd=pd, f=f, dc=dc, c0=c0, Wd=Wd: te.matmul(
                                    banks[pd][:, :], lhsT=Wd[:, f, dc * 128:(dc + 1) * 128], rhs=act[:, f, c0:c0 + 512],
                                    start=(f == 0), stop=(f == 3))), reads=[self.Wd_b[s], self.act_b[f][tg]], writes=bk(pd))
                            add_to_x(pd, 512, dc, tg * 4)

        def ffn0_phase():
            sc.barrier()
            A.reset()
            F = FFN(False)
            sq = [A.b(512) for _ in range(2)]
            sq_b = [Buf("sq0"), Buf("sq1")]
            rs = A.f(512)
            rs_b = Buf("rs")
            for g in range(4):
                norm_to(512, g * 4, V_NF + 0, lambda c, g=g: F.hT[:, c, g * 512:(g + 1) * 512], lambda c, g=g: [F.hT_b[c][g]],
                        sq, sq_b, 6 + g % 2, rs, rs_b)
            F.run(ffn_w_gate, ffn_w_up, ffn_w_down)

        def moba_phase():
            sc.barrier()
            A.reset()
            kT = A.b(8, S)
            Vv = A.b(16, 1024)
            Wkv = A.b(8, 2048)
            hTg = A.b(8, 512)
            sq = [A.b(512) for _ in range(2)]
            qn = A.b(8, 256)
            onT = A.b(8, 256)
            NP = 4
            Pb = [A.b(256) for _ in range(NP)]
            nselT = A.b(8, 256)
            rs2 = [A.f(512) for _ in range(2)]
            knf = [A.f(512) for _ in range(2)]
            ksum = A.f(8, 8)
            qnf = [knf[i][:, 0:256] for i in range(2)]
            gsb = A.f(16, 8)
            cmpb = A.f(16, 8, 8)
            rank = A.f(16, 8)
            nsel = A.f(16, 8)
            rden = [A.f(256) for _ in range(2)]
            kT_b = [[Buf(f"kT{h}_{g}") for g in range(4)] for h in range(8)]
            Vv_b = [Buf(f"V{t}") for t in range(16)]
            Wk_b = [Buf(f"Wkv{k}") for k in range(8)]
            hTg_b = Buf("hTg")
            sq_b = [Buf("sq0"), Buf("sq1")]
            qn_b = [Buf(f"qn{h}") for h in range(8)]
            onT_b = [Buf(f"onT{h}") for h in range(8)]
            Pb_b = [Buf(f"P{i}") for i in range(NP)]
            nselT_b = [Buf(f"nselT{h}") for h in range(8)]
            rs2_b = [Buf("rs0"), Buf("rs1")]
            knf_b = [Buf("knf0"), Buf("knf1")]
            ksum_b = Buf("ksum")
            qnf_b = knf_b
            gsb_b, cmpb_b, rank_b, nsel_b = Buf("gsb"), Buf("cmpb"), Buf("rank"), Buf("nsel")
            rden_b = [Buf("rden0"), Buf("rden1")]
            rA = Rot([0, 1])
            rB = Rot([2, 3, 4])
            for k in range(8):
                load_w(Wkv[:, k, :], Wk_b[k], moba_w_qkv[k * 128:(k + 1) * 128, 1024:3072], f"Wkv{k}")
            sc.op(POOL, lambda: ge.memset(nselT, 0.0), writes=nselT_b)

            rB4 = Rot([2, 3, 4, 5])

            def head_norm_pipeline(nheads, n, proj_mm, gain_col, finish, rot=None, filler=None):
                rot = rot or rB
                pend = None
                for h in range(nheads + 1):
                    cur = None
                    if h < nheads:
                        pk = rot.next()
                        proj_mm(h, pk)
                        s = h % 2
                        sc.op(ACT, (lambda pk=pk, s=s: se.activation(out=sq[s][:, 0:n], in_=banks[pk][:, 0:n], func=AF.Square)),
                              reads=bk(pk), writes=[sq_b[s]])
                        cur = (h, pk, s)
                        if filler is not None:
                            filler(h)
                    if pend is not None:
                        ph, ppk, ps = pend
                        bs = rA.next()
                        sc.op(PE, (lambda bs=bs, ps=ps: te.matmul(banks[bs][:, 0:n], lhsT=ones_b, rhs=sq[ps][:, 0:n], start=True, stop=True)),
                              reads=[sq_b[ps], cB_b], writes=bk(bs))
                        sc.op(ACT, (lambda bs=bs, ps=ps: se.activation(out=rs2[ps][:, 0:n], in_=banks[bs][:, 0:n], func=AF.Ln,
                                                                        bias=EPS, scale=1.0 / 128.0)),
                              reads=bk(bs), writes=[rs2_b[ps]])
                        sc.op(ACT, (lambda ps=ps: se.activation(out=rs2[ps][:, 0:n], in_=rs2[ps][:, 0:n], func=AF.Exp, scale=-0.5)),
                              reads=[rs2_b[ps]], writes=[rs2_b[ps]])
                        finish(ph, ppk, ps)
                    pend = cur

            for g in range(4):
                norm_to(512, g * 4, V_NM + 8, lambda c: hTg[:, c, :], lambda c: [hTg_b], sq, sq_b, rA.next(), rs2[0], rs2_b[0])

                def k_proj(h, pk):
                    for k in range(8):
                        sc.op(PE, (lambda pk=pk, k=k, h=h: te.matmul(banks[pk][:, :], lhsT=Wkv[:, k, h * 128:(h + 1) * 128], rhs=hTg[:, k, :],
                                                                     start=(k == 0), stop=(k == 7))),
                              reads=[Wk_b[k], hTg_b], writes=bk(pk))

                def k_finish(h, pk, s, g=g):
                    sc.op(DVE, (lambda pk=pk, s=s: ve.scalar_tensor_tensor(out=knf[s], in0=banks[pk][:, :], scalar=vecs[:, V_KG:V_KG + 1],
                                                                           in1=rs2[s], op0=ALU.mult, op1=ALU.mult)),
                          reads=bk(pk) + [rs2_b[s], vecs_b], writes=[knf_b[s]])
                    sc.op(POOL, (lambda s=s, h=h, g=g: ge.tensor_copy(out=kT[:, h, g * 512:(g + 1) * 512], in_=knf[s])),
                          reads=[knf_b[s]], writes=[kT_b[h][g]])
                    sc.op(DVE, (lambda s=s, h=h, g=g: ve.tensor_reduce(out=ksum[:, h, 2 * g:2 * g + 2],
                                                                       in_=knf[s].rearrange("p (a b) -> p a b", a=2),
                                                                       axis=AX.X, op=ALU.add)),
                          reads=[knf_b[s]], writes=[ksum_b])

                def v_fill(h, g=g):
                    t, half = h // 2, h % 2
                    tt = g * 4 + t
                    pv = rA.next()
                    for k in range(8):
                        sc.op(PE, (lambda pv=pv, k=k, t=t, half=half: te.matmul(
                            banks[pv][:, :], lhsT=hTg[:, k, t * 128:(t + 1) * 128], rhs=Wkv[:, k, 1024 + half * 512:1024 + (half + 1) * 512],
                            start=(k == 0), stop=(k == 7))), reads=[Wk_b[k], hTg_b], writes=bk(pv))
                    sc.op(DVE, (lambda pv=pv, tt=tt, half=half: ve.tensor_copy(out=Vv[:, tt, half * 512:(half + 1) * 512], in_=banks[pv][:, :])),
                          reads=bk(pv), writes=[Vv_b[tt]])

                head_norm_pipeline(8, 512, k_proj, V_KG, k_finish, rot=rB4, filler=v_fill)
            Wq = Wkv[:, :, 0:1024]
            Wo = Wkv[:, :, 1024:2048]
            Wq_b = [Buf(f"Wq{k}") for k in range(8)]
            Wo_b = Buf("Wo")
            for k in range(8):
                sc.op(POOL, (lambda k=k: ge.dma_start(out=Wq[:, k, :], in_=moba_w_qkv[k * 128:(k + 1) * 128, 0:1024])),
                      reads=[], writes=[Wq_b[k], Wk_b[k]], dma_key=f"Wq{k}")
            sc.op(POOL, lambda: ge.dma_start(out=Wo, in_=moba_w_out.rearrange("(k p) n -> p k n", p=128)),
                  writes=[Wo_b] + Wk_b, dma_key="Wo")
            hTb2 = [hTg[:, :, 0:256], hTg[:, :, 256:512]]
            hTb2_b = [Buf("hTb0"), Buf("hTb1")]
            rS = Rot([2, 3, 4])
            rP = Rot(list(range(NP)))
            rQ = Rot([5, 1])
            LA = 2
            scale = 128.0 ** -0.5
            sq256 = [x[:, 0:256] for x in sq]

            def q_norm(b):
                norm_to(256, b * 2, V_NM + 8, lambda c: hTb2[b % 2][:, c, :], lambda c: [hTb2_b[b % 2], hTg_b], sq256, sq_b, 0,
                        rs2[0][:, 0:256], rs2_b[0])

            class QPipe:
                def __init__(self, b):
                    self.b = b
                    self.hT = hTb2[b % 2]
                    self.hT_b = hTb2_b[b % 2]
                    self.pend = None
                    self.gate_pend = None
                    self.h = 0

                def step(self):
                    b, h = self.b, self.h
                    cur = None
                    if self.gate_pend is not None:
                        gh, gs = self.gate_pend
                        for tt in range(2):
                            m = gh * 2 + tt
                            sc.op(PE, (lambda gs=gs, tt=tt, gh=gh, m=m: te.matmul(banks[0][:, 384 + m * 8:384 + (m + 1) * 8],
                                                                                  lhsT=qnf[gs][:, tt * 128:(tt + 1) * 128],
                                                                                  rhs=ksum[:, gh, :], start=True, stop=True)),
                                  reads=[qnf_b[gs], ksum_b], writes=bk(0))
                        self.gate_pend = None
                    if h < 8:
                        pq = rQ.next()
                        for k in range(8):
                            sc.op(PE, (lambda pq=pq, k=k, h=h, hT=self.hT: te.matmul(banks[pq][:, 0:256], lhsT=Wq[:, k, h * 128:(h + 1) * 128],
                                                                                   rhs=hT[:, k, :], start=(k == 0), stop=(k == 7))),
                                  reads=[Wq_b[k], self.hT_b], writes=bk(pq))
                        s = h % 2
                        sc.op(ACT, (lambda pq=pq, s=s: se.activation(out=sq256[s], in_=banks[pq][:, 0:256], func=AF.Square)),
                              reads=bk(pq), writes=[sq_b[s]])
                        cur = (h, pq, s)
                    if self.pend is not None:
                        ph, ppq, ps = self.pend
                        sc.op(PE, (lambda ps=ps: te.matmul(banks[0][:, 0:256], lhsT=ones_b, rhs=sq256[ps], start=True, stop=True)),
                              reads=[sq_b[ps], cB_b], writes=bk(0))
                        sc.op(ACT, (lambda ps=ps: se.activation(out=rs2[ps][:, 0:256], in_=banks[0][:, 0:256], func=AF.Ln, bias=EPS, scale=1.0 / 128.0)),
                              reads=bk(0), writes=[rs2_b[ps]])
                        sc.op(ACT, (lambda ps=ps: se.activation(out=rs2[ps][:, 0:256], in_=rs2[ps][:, 0:256], func=AF.Exp, scale=-0.5)),
                              reads=[rs2_b[ps]], writes=[rs2_b[ps]])
                        sc.op(DVE, (lambda ppq=ppq, ps=ps: ve.scalar_tensor_tensor(out=qnf[ps], in0=banks[ppq][:, 0:256], scalar=vecs[:, V_QG:V_QG + 1],
                                                                                   in1=rs2[ps][:, 0:256], op0=ALU.mult, op1=ALU.mult)),
                              reads=bk(ppq) + [rs2_b[ps], vecs_b], writes=[qnf_b[ps]])
                        sc.op(POOL, (lambda ps=ps, ph=ph: ge.tensor_copy(out=qn[:, ph, :], in_=qnf[ps])), reads=[qnf_b[ps]], writes=[qn_b[ph]])
                        if b >= 4:
                            self.gate_pend = (ph, ps)
                    self.pend = cur
                    self.h += 1

                def done(self):
                    return self.h > 9

            def gating(b):
                if b < 4:
                    return
                sc.op(DVE, lambda: ve.tensor_copy(out=gsb.rearrange("p a b -> p (a b)"), in_=banks[0][:, 384:512]), reads=bk(0), writes=[gsb_b])
                sc.op(DVE, (lambda b=b: ve.tensor_tensor(out=cmpb[:, :, :, 0:b],
                                                        in0=gsb[:, :, 0:b].unsqueeze(2).to_broadcast([128, 16, 8, b]),
                                                        in1=gsb[:, :, :].unsqueeze(3).to_broadcast([128, 16, 8, b]), op=ALU.is_gt)),
                      reads=[gsb_b], writes=[cmpb_b])
                sc.op(DVE, (lambda b=b: ve.tensor_reduce(out=rank, in_=cmpb[:, :, :, 0:b], axis=AX.X, op=ALU.add)),
                      reads=[cmpb_b], writes=[rank_b])
                sc.op(DVE, lambda: ve.tensor_scalar(out=nsel, in0=rank, scalar1=3.0, scalar2=NEG, op0=ALU.is_ge, op1=ALU.mult),
                      reads=[rank_b], writes=[nsel_b])
                for j in range(4):
                    bt = rA.next()
                    for q in range(4):
                        m = 4 * j + q
                        sc.op(PE, (lambda bt=bt, q=q, m=m: te.matmul(banks[bt][0:8, q * 128:(q + 1) * 128], lhsT=nsel[:, m, :], rhs=ident_f,
                                                                     start=True, stop=True)),
                              reads=[nsel_b, cF_b], writes=bk(bt))
                    sc.op(ACT, (lambda bt=bt, j=j: se.copy(out=nselT[0:8, 2 * j:2 * j + 2, :].rearrange("p a b -> p (a b)"), in_=banks[bt][0:8, :])),
                          reads=bk(bt), writes=[nselT_b[2 * j], nselT_b[2 * j + 1]])

            q_norm(0)
            qp = QPipe(0)
            while not qp.done():
                qp.step()
            for b in range(8):
                T0 = b * 2
                nxt = None
                if b + 1 < 8:
                    q_norm(b + 1)
                    nxt = QPipe(b + 1)
                nkt = 2 * b + 2
                items = [(h, kt) for h in range(8) for kt in range(nkt)]
                info = {}
                for idx in range(len(items) + LA):
                    if idx < len(items):
                        h, kt = items[idx]
                        kb = kt // 2
                        sbk = rS.next()
                        Sap = banks[sbk][:, 0:256]
                        Sbuf = bk(sbk)
                        own = kb == b
                        a = kt - 2 * b
                        q0 = 128 if (own and a == 1) else 0
                        need_sel = (not own) and b >= 4
                        sc.op(PE, (lambda Sap=Sap, kt=kt, h=h, q0=q0, own=own, need_sel=need_sel: te.matmul(
                            Sap[:, q0:256], lhsT=kT[:, h, kt * 128:(kt + 1) * 128], rhs=qn[:, h, q0:256],
                            start=True, stop=not (own or need_sel))), reads=[kT_b[h][kt // 4], qn_b[h]], writes=Sbuf)
                        if need_sel:
                            sc.op(PE, (lambda Sap=Sap, kb=kb, h=h: te.matmul(Sap, lhsT=ind_full(kb), rhs=nselT[:, h, :], start=False, stop=True)),
                                  reads=[cB_b, nselT_b[h]], writes=Sbuf)
                        if own:
                            sc.op(PE, (lambda Sap=Sap, q0=q0: te.matmul(Sap[:, q0:q0 + 128], lhsT=ident_b, rhs=cmask_b, start=False, stop=True)),
                                  reads=[cB_b], writes=Sbuf)
                        pi = rP.next()
                        sc.op(ACT, (lambda Sap=Sap, pi=pi, q0=q0: se.activation(out=Pb[pi][:, q0:256], in_=Sap[:, q0:256], func=AF.Exp, scale=scale)),
                              reads=Sbuf, writes=[Pb_b[pi]])
                        info[idx] = (pi, q0)
                    if idx >= LA:
                        h, kt = items[idx - LA]
                        pi, q0 = info.pop(idx - LA)
                        ob = 6 + h % 2
                        lastk = kt == nkt - 1
                        sc.op(PE, (lambda ob=ob, pi=pi, kt=kt, h=h, q0=q0: te.matmul(
                            banks[ob][:, q0:256], lhsT=Vv[:, kt, h * 128:(h + 1) * 128], rhs=Pb[pi][:, q0:256],
                            start=(kt == 0), stop=False)), reads=[Vv_b[kt], Pb_b[pi]], writes=bk(ob))
                        sc.op(PE, (lambda ob=ob, pi=pi, kt=kt, q0=q0, lastk=lastk: te.matmul(
                            banks[ob][:, 256 + q0:512], lhsT=ones_b, rhs=Pb[pi][:, q0:256],
                            start=False, stop=lastk)), reads=[cB_b, Pb_b[pi]], writes=bk(ob))
                        if lastk:
                            r = h % 2
                            sc.op(DVE, (lambda ob=ob, r=r: ve.reciprocal(out=rden[r], in_=banks[ob][:, 256:512])), reads=bk(ob), writes=[rden_b[r]])
                            sc.op(DVE, (lambda ob=ob, r=r, h=h: ve.tensor_tensor(out=onT[:, h, :], in0=banks[ob][:, 0:256], in1=rden[r], op=ALU.mult)),
                                  reads=bk(ob) + [rden_b[r]], writes=[onT_b[h]])
                            if nxt is not None:
                                nxt.step()
                if nxt is not None:
                    while not nxt.done():
                        nxt.step()
                    gating(b + 1)
                for dc in range(8):
                    bo = rA.next()
                    for h in range(8):
                        sc.op(PE, (lambda h=h, dc=dc, bo=bo: te.matmul(banks[bo][:, 0:256], lhsT=Wo[:, h, dc * 128:(dc + 1) * 128], rhs=onT[:, h, :],
                                                                       start=(h == 0), stop=(h == 7))),
                              reads=[Wo_b, onT_b[h]], writes=bk(bo))
                    add_to_x(bo, 256, dc, T0)

        def moe_phase():
            sc.barrier()
            A.reset()
            F = FFN(True)
            sq = [A.b(512) for _ in range(2)]
            sq_b = [Buf("sq0"), Buf("sq1")]
            rs = A.f(512)
            rs_b = Buf("rs")
            hf = [A.f(512) for _ in range(2)]
            hf_b = [Buf("hf0"), Buf("hf1")]
            Wr = A.f(8, 8)
            Wr_b = Buf("Wr")
            gates = A.f(16, 8)
            gates_b = [Buf(f"gates{t}") for t in range(16)]
            lgall = A.f(16, 8); dd = A.f(16, 8); eq = A.f(16, 8); lg2 = A.f(16, 8); selm = A.f(16, 8); ex = A.f(16, 8)
            m1 = A.f(16); m2 = A.f(16); den = A.f(16)
            lgall_b, dd_b, eq_b, lg2_b, selm_b, ex_b, m1_b, m2_b, den_b = [Buf(n) for n in ("lgall", "dd", "eq", "lg2", "selm", "ex", "m1", "m2", "den")]
            gall_b = Buf("gates")
            gbc = [A.f(S) for _ in range(2)]
            gbc_b = [[Buf(f"gbc{i}_{g}") for g in range(4)] for i in range(2)]
            sc.op(SP, lambda: sy.dma_start(out=Wr, in_=moe_w_router.rearrange("(k p) n -> p k n", p=128)), writes=[Wr_b], dma_key="Wr")
            for g in range(4):
                T0 = g * 4
                c0 = g * 512
                rms_rstd(lambda c, c0=c0: xT[:, c, c0:c0 + 512], lambda c: xbufs([c], T0, T0 + 4), 8, 512, sq, sq_b, 6, rs, rs_b, 1.0 / D)
                for c in range(8):
                    s = c % 2
                    sc.op(DVE, (lambda c=c, s=s, c0=c0: ve.scalar_tensor_tensor(out=hf[s], in0=xT[:, c, c0:c0 + 512],
                                                                         scalar=vecs[:, V_NF + 8 + c:V_NF + 8 + c + 1], in1=rs,
                                                                         op0=ALU.mult, op1=ALU.mult)),
                          reads=xbufs([c], T0, T0 + 4) + [rs_b, vecs_b], writes=[hf_b[s]])
                    sc.op(ACT, (lambda c=c, s=s, c0=c0: se.copy(out=F.hT[:, c, c0:c0 + 512], in_=hf[s])), reads=[hf_b[s]], writes=[F.hT_b[c][g]])
                    for tt in range(4):
                        sc.op(PE, (lambda c=c, s=s, tt=tt: te.matmul(banks[7][:, tt * 8:(tt + 1) * 8], lhsT=hf[s][:, tt * 128:(tt + 1) * 128],
                                                                     rhs=Wr[:, c, :], start=(c == 0 and tt == 0), stop=(c == 7 and tt == 3))),
                              reads=[hf_b[s], Wr_b], writes=[bk(7)[0]])
                sc.op(DVE, (lambda g=g: ve.tensor_copy(out=lgall[:, 4 * g:4 * g + 4, :].rearrange("p a b -> p (a b)"), in_=banks[7][:, 0:32])),
                      reads=[bk(7)[0]], writes=[lgall_b])
            bc3 = lambda v: v.unsqueeze(2).to_broadcast([128, 16, 8])
            sc.op(DVE, lambda: ve.tensor_reduce(out=m1, in_=lgall, axis=AX.X, op=ALU.max), reads=[lgall_b], writes=[m1_b])
            sc.op(DVE, lambda: ve.tensor_tensor(out=dd, in0=lgall, in1=bc3(m1), op=ALU.subtract), reads=[lgall_b, m1_b], writes=[dd_b])
            sc.op(DVE, lambda: ve.tensor_scalar(out=eq, in0=dd, scalar1=0.0, scalar2=None, op0=ALU.is_ge), reads=[dd_b], writes=[eq_b])
            sc.op(DVE, lambda: ve.scalar_tensor_tensor(out=lg2, in0=eq, scalar=-1e30, in1=dd, op0=ALU.mult, op1=ALU.add),
                  reads=[eq_b, dd_b], writes=[lg2_b])
            sc.op(DVE, lambda: ve.tensor_reduce(out=m2, in_=lg2, axis=AX.X, op=ALU.max), reads=[lg2_b], writes=[m2_b])
            sc.op(DVE, lambda: ve.tensor_tensor(out=selm, in0=dd, in1=bc3(m2), op=ALU.is_ge), reads=[dd_b, m2_b], writes=[selm_b])
            sc.op(ACT, lambda: se.activation(out=ex, in_=dd, func=AF.Exp), reads=[dd_b], writes=[ex_b])
            sc.op(DVE, lambda: ve.tensor_tensor(out=ex, in0=ex, in1=selm, op=ALU.mult), reads=[ex_b, selm_b], writes=[ex_b])
            sc.op(DVE, lambda: ve.tensor_reduce(out=den, in_=ex, axis=AX.X, op=ALU.add), reads=[ex_b], writes=[den_b])
            sc.op(DVE, lambda: ve.reciprocal(out=den, in_=den), reads=[den_b], writes=[den_b])
            sc.op(DVE, lambda: ve.tensor_tensor(out=gates, in0=ex, in1=bc3(den), op=ALU.mult), reads=[ex_b, den_b], writes=[gall_b])

            def build_gbc(e):
                gi = e % 2
                for g in range(4):
                    pb = 6 + g % 2
                    for tt in range(4):
                        t = g * 4 + tt
                        sc.op(PE, (lambda pb=pb, tt=tt, t=t, e=e: te.matmul(banks[pb][:, tt * 128:(tt + 1) * 128],
                                                                            lhsT=gates[:, t, e:e + 1].to_broadcast([128, 128]), rhs=ident_f,
                                                                            start=True, stop=True)),
                              reads=[gall_b, cF_b], writes=bk(pb))
                    sc.op(ACT, (lambda pb=pb, gi=gi, g=g: se.copy(out=gbc[gi][:, g * 512:(g + 1) * 512], in_=banks[pb][:, :])),
                          reads=bk(pb), writes=[gbc_b[gi][g]])

            build_gbc(0)
            for e in range(NE):
                gi = e % 2
                hook = (lambda e=e: build_gbc(e + 1)) if e + 1 < NE else None
                F.run(moe_w_gate[e], moe_w_up[e], moe_w_down[e], gbc=gbc[gi], gbc_b=gbc_b[gi], mid_hook=hook)

        if stage >= 1:
            gla_phase()
        if stage >= 2:
            ffn0_phase()
        if stage >= 3:
            moba_phase()
        if stage >= 4:
            moe_phase()

        sc.barrier()
        A.reset()
        xo = [A.f(1024) for i in range(4)]
        xo_b = [Buf(f"xo{i}") for i in range(4)]
        for t in range(16):
            s = t % 4
            for half in range(2):
                b = 2 * (t % 4) + half
                for q in range(4):
                    c = half * 4 + q
                    sc.op(PE, (lambda b=b, q=q, c=c, t=t: te.transpose(banks[b][:, q * 128:(q + 1) * 128],
                                                                      xT[:, c, t * 128:(t + 1) * 128], ident_f)),
                          reads=[xT_b[c][t], cF_b], writes=bk(b))
                if half == 0:
                    sc.op(ACT, (lambda b=b, s=s: se.copy(out=xo[s][:, 0:512], in_=banks[b][:, :])), reads=bk(b), writes=[xo_b[s]])
                else:
                    sc.op(DVE, (lambda b=b, s=s: ve.tensor_copy(out=xo[s][:, 512:1024], in_=banks[b][:, :])), reads=bk(b), writes=[xo_b[s]])
            sc.op(SP, (lambda s=s, t=t: sy.dma_start(out=out_d[t * 128:(t + 1) * 128, :], in_=xo[s])),
                  reads=[xo_b[s]], dma_key=f"xo{s}")
        sc.op(SP, None, writes=xo_b)

        block = es.enter_context(nc.Block())
        sc.emit(nc, es, block)
    return nc


_CACHE = {}


def _layout_vecs(inputs):
    v = np.zeros((128, NV), np.float32)
    nm = np.asarray(inputs["norm_mix"], np.float32)
    nf = np.asarray(inputs["norm_ffn"], np.float32)
    for i in range(2):
        v[:, V_NM + i * 8:V_NM + (i + 1) * 8] = nm[i].reshape(8, 128).T
        v[:, V_NF + i * 8:V_NF + (i + 1) * 8] = nf[i].reshape(8, 128).T
    v[:, V_OG:V_OG + 2] = np.asarray(inputs["gla_out_gain"], np.float32)[0].reshape(2, 128).T
    v[:, V_QG] = np.asarray(inputs["moba_q_gain"], np.float32)[0]
    v[:, V_KG] = np.asarray(inputs["moba_k_gain"], np.float32)[0]
    return v


def kernel(_stage=4, _ncores=8, **inputs):
    if _stage not in _CACHE:
        _CACHE[_stage] = build(_stage)
    nc = _CACHE[_stage]
    f = lambda k: np.ascontiguousarray(np.asarray(inputs[k], np.float32))
    x = f("x")
    cf, cb = host_consts()
    shared = {
        "constsf": cf, "constsb": cb,
        "vecs": _layout_vecs(inputs),
        "gla_w_in": f("gla_w_in")[0], "gla_w_gate2": f("gla_w_gate2")[0], "gla_b_gate": f("gla_b_gate"),
        "gla_w_out": f("gla_w_out")[0], "moba_w_qkv": f("moba_w_qkv")[0], "moba_w_out": f("moba_w_out")[0],
        "ffn_w_gate": f("ffn_w_gate")[0], "ffn_w_up": f("ffn_w_up")[0], "ffn_w_down": f("ffn_w_down")[0],
        "moe_w_router": f("moe_w_router")[0], "moe_w_gate": f("moe_w_gate")[0], "moe_w_up": f("moe_w_up")[0],
        "moe_w_down": f("moe_w_down")[0],
    }
    in_maps = []
    for b in range(_ncores):
        m = dict(shared)
        m["x"] = x[b]
        in_maps.append(m)
    res = run_bass_kernel_spmd(nc, in_maps, core_ids=list(range(_ncores)))
    return np.stack([np.asarray(r["out"], np.float32) for r in res.results], axis=0)
```
